# Optimizing a Trainium2 kernel written in Bass

```python
import jax, jax.numpy as jnp
from jax import lax
import numpy as np

D_MODEL = 4096
BATCH = 4
SEQ = 2048
DEPTH = 1

N_HEADS = 16
HEAD_DIM = 128
N_KV_HEADS = 4
ATT_WIDTH = N_HEADS * HEAD_DIM
KV_WIDTH = N_KV_HEADS * HEAD_DIM
IDX_HEADS = 32
IDX_DIM = 64
TOPK_MAX = 256
Q_BLOCK = 128
GM_WIDTH = 2048
GM_GROUPS = 8
GM_GROUP_W = GM_WIDTH // GM_GROUPS
GM_CHUNK = 128
D_FF = 11008
CONV_W = 3
EPS = 1e-6
NEG_BIG = -1e30
IN_SIZES = (ATT_WIDTH, KV_WIDTH, KV_WIDTH, IDX_HEADS * IDX_DIM, IDX_DIM, IDX_HEADS,
            GM_WIDTH, GM_WIDTH, D_MODEL, D_MODEL)
IN_WIDTH = (ATT_WIDTH + 2 * KV_WIDTH + IDX_HEADS * IDX_DIM + IDX_DIM + IDX_HEADS
            + 2 * GM_WIDTH + 2 * D_MODEL)

kernel_name = 'hybrid_dsa_gmlp_convffn_adaln'


def _split_points():
    return np.cumsum(np.array(IN_SIZES))[:-1].tolist()


def _rmsnorm(x, g):
    xf = x.astype(jnp.float32)
    y = xf * lax.rsqrt(jnp.mean(xf * xf, axis=-1, keepdims=True) + EPS)
    return (y * g.astype(jnp.float32)).astype(x.dtype)


def _alibi_slopes(n):
    return jnp.asarray([2.0 ** (-8.0 * (i + 1) / n) for i in range(n)], dtype=jnp.float32)


def _dsa_attention(q, k, v, q_idx, k_idx, w_idx):
    B, S = q.shape[0], q.shape[1]
    topk = min(TOPK_MAX, S // 4)
    n_blk = S // Q_BLOCK
    rep = N_HEADS // N_KV_HEADS
    slopes = _alibi_slopes(N_HEADS).reshape(N_KV_HEADS, rep)
    key_pos = jnp.arange(S, dtype=jnp.int32)
    scale = HEAD_DIM ** -0.5
    idx_scale = IDX_DIM ** -0.5
    w_scale = IDX_HEADS ** -0.5

    def block(i):
        t0 = i * Q_BLOCK
        qb = lax.dynamic_slice_in_dim(q, t0, Q_BLOCK, axis=1).reshape(B, Q_BLOCK, N_KV_HEADS, rep, HEAD_DIM)
        qib = lax.dynamic_slice_in_dim(q_idx, t0, Q_BLOCK, axis=1)
        wb = lax.dynamic_slice_in_dim(w_idx, t0, Q_BLOCK, axis=1)
        q_pos = t0 + jnp.arange(Q_BLOCK, dtype=jnp.int32)
        causal = key_pos[None, :] <= q_pos[:, None]
        logits = jnp.einsum('bthd,bsd->btsh', qib, k_idx).astype(jnp.float32) * idx_scale
        iscore = jnp.einsum('btsh,bth->bts', jax.nn.relu(logits), wb.astype(jnp.float32) * w_scale)
        iscore = jnp.where(causal[None], iscore, -jnp.inf)
        _, sel = lax.top_k(iscore, topk)
        valid = sel <= q_pos[None, :, None]
        k_sel = jax.vmap(lambda kk, ii: kk[ii])(k, sel)
        v_sel = jax.vmap(lambda vv, ii: vv[ii])(v, sel)
        s = jnp.einsum('btgrd,btkgd->btgrk', qb, k_sel).astype(jnp.float32) * scale
        dist = (q_pos[None, :, None] - sel).astype(jnp.float32)
        s = s - slopes[None, None, :, :, None] * dist[:, :, None, None, :]
        s = jnp.where(valid[:, :, None, None, :], s, NEG_BIG)
        p = jax.nn.softmax(s, axis=-1).astype(v.dtype)
        o = jnp.einsum('btgrk,btkgd->btgrd', p, v_sel)
        return o.reshape(B, Q_BLOCK, ATT_WIDTH)

    out = lax.map(block, jnp.arange(n_blk, dtype=jnp.int32))
    return out.transpose(1, 0, 2, 3).reshape(B, S, ATT_WIDTH)


def _chunked_sgu(u, v, g, w_s, b_s):
    B, S = u.shape[0], u.shape[1]
    n = S // GM_CHUNK
    vn = _rmsnorm(v, g).reshape(B, n, GM_CHUNK, GM_GROUPS, GM_GROUP_W)
    mask = jnp.tril(jnp.ones((GM_CHUNK, GM_CHUNK), dtype=bool))
    w = jnp.where(mask[None], w_s, jnp.zeros_like(w_s))
    f = jnp.einsum('gts,bnsgc->bntgc', w, vn) + b_s.T[None, None, :, :, None]
    return u * f.reshape(B, S, GM_WIDTH)


def _conv_ffn(h, w_up, conv_w, conv_b, w_down):
    S = h.shape[1]
    a = h @ w_up
    ap = jnp.pad(a, ((0, 0), (CONV_W - 1, 0), (0, 0)))
    acc = conv_b
    for j in range(CONV_W):
        acc = acc + ap[:, j:j + S] * conv_w[j]
    gate, val = jnp.split(acc, 2, axis=-1)
    return (jax.nn.silu(gate) * val) @ w_down


def setup_inputs(seed: int = 0) -> dict:
    key = jax.random.key(seed)
    ks = jax.random.split(key, 20)
    f32 = jnp.float32
    nrm = lambda k, shape, s: jax.random.normal(k, shape, f32) * s
    L = DEPTH
    return {
        'x': nrm(ks[0], (BATCH, SEQ, D_MODEL), 1.0),
        'c': nrm(ks[1], (BATCH, D_MODEL), 1.0),
        'ada_w': nrm(ks[2], (L, D_MODEL, 6 * D_MODEL), 0.5 * D_MODEL ** -0.5),
        'ada_b': nrm(ks[3], (L, 6 * D_MODEL), 0.01),
        'norm1_g': 1.0 + nrm(ks[4], (L, D_MODEL), 0.02),
        'w_in': nrm(ks[5], (L, D_MODEL, IN_WIDTH), D_MODEL ** -0.5),
        'q_norm_g': 1.0 + nrm(ks[6], (L, HEAD_DIM), 0.02),
        'k_norm_g': 1.0 + nrm(ks[7], (L, HEAD_DIM), 0.02),
        'sgu_norm_g': 1.0 + nrm(ks[8], (L, GM_WIDTH), 0.02),
        'sgu_w': nrm(ks[9], (L, GM_GROUPS, GM_CHUNK, GM_CHUNK), GM_CHUNK ** -0.5),
        'sgu_b': 1.0 + nrm(ks[10], (L, GM_GROUPS, GM_CHUNK), 0.1),
        'w_branch_a': nrm(ks[11], (L, ATT_WIDTH, D_MODEL), ATT_WIDTH ** -0.5),
        'w_branch_b': nrm(ks[12], (L, GM_WIDTH, D_MODEL), GM_WIDTH ** -0.5),
        'w_out': nrm(ks[13], (L, D_MODEL, D_MODEL), D_MODEL ** -0.5),
        'norm2_g': 1.0 + nrm(ks[14], (L, D_MODEL), 0.02),
        'w_up': nrm(ks[15], (L, D_MODEL, 2 * D_FF), D_MODEL ** -0.5),
        'conv_w': nrm(ks[16], (L, CONV_W, 2 * D_FF), CONV_W ** -0.5),
        'conv_b': nrm(ks[17], (L, 2 * D_FF), 0.01),
        'w_down': nrm(ks[18], (L, D_FF, D_MODEL), D_FF ** -0.5),
    }


def reference(x, c, ada_w, ada_b, norm1_g, w_in, q_norm_g, k_norm_g, sgu_norm_g, sgu_w, sgu_b,
              w_branch_a, w_branch_b, w_out, norm2_g, w_up, conv_w, conv_b, w_down):
    B, S = x.shape[0], x.shape[1]
    cs = jax.nn.silu(c)
    splits = _split_points()
    for l in range(DEPTH):
        mod = cs @ ada_w[l] + ada_b[l]
        sh1, sc1, g1, sh2, sc2, g2 = [m[:, None, :] for m in jnp.split(mod, 6, axis=-1)]
        h = _rmsnorm(x, norm1_g[l]) * (1 + sc1) + sh1
        proj = h @ w_in[l]
        q, k, v, qi, ki, wi, gu, gv, ga, gb = jnp.split(proj, splits, axis=-1)
        q = _rmsnorm(q.reshape(B, S, N_HEADS, HEAD_DIM), q_norm_g[l])
        k = _rmsnorm(k.reshape(B, S, N_KV_HEADS, HEAD_DIM), k_norm_g[l])
        v = v.reshape(B, S, N_KV_HEADS, HEAD_DIM)
        qi = qi.reshape(B, S, IDX_HEADS, IDX_DIM)
        y_a = _dsa_attention(q, k, v, qi, ki, wi)
        y_b = _chunked_sgu(jax.nn.gelu(gu, approximate=False), jax.nn.gelu(gv, approximate=False),
                           sgu_norm_g[l], sgu_w[l], sgu_b[l])
        merged = jax.nn.sigmoid(ga) * (y_a @ w_branch_a[l]) + jax.nn.sigmoid(gb) * (y_b @ w_branch_b[l])
        x = x + g1 * (merged @ w_out[l])
        h2 = _rmsnorm(x, norm2_g[l]) * (1 + sc2) + sh2
        x = x + g2 * _conv_ffn(h2, w_up[l], conv_w[l], conv_b[l], w_down[l])
    return x
```

```python
import numpy as np
import concourse.bass as bass
import concourse.mybir as mybir
from concourse.bass_utils import run_bass_kernel_spmd

F32 = mybir.dt.float32
BF16 = mybir.dt.bfloat16
AF = mybir.ActivationFunctionType
ALU = mybir.AluOpType
AX = mybir.AxisListType

D = 4096
KC = 32
SEQ = 2048
NB = 4
IN_W = 17504
DFF = 11008
NFF = 86
TE = 1152
FMC = [(126, 342), (468, 342), (810, 342)]
C_Q, C_K, C_V, C_QI, C_KI, C_WI, C_GU, C_GV, C_GA, C_GB = 0, 2048, 2560, 3072, 5120, 5184, 5216, 7264, 9312, 13408
EPS = 1e-6
NEG = -1.0e30
BIGD = 1.0e7
SCALE = 128.0 ** -0.5
IDXS = (64.0 ** -0.5) * (32.0 ** -0.5)
SLOPES = [2.0 ** (-8.0 * (i + 1) / 16) for i in range(16)]
ARENA = 212000


class Buf:
    __slots__ = ("name", "writers", "readers", "war", "excl", "last")

    def __init__(self, name, excl=False):
        self.name = name
        self.writers = []
        self.readers = {}
        self.war = set()
        self.excl = excl
        self.last = {}


class Sem:
    def __init__(self, h):
        self.h = h
        self.count = 0


class Op:
    __slots__ = ("eng", "fn", "wdeps", "rdeps", "sig", "idx", "dsem", "dval")


ENGS = ("pe", "act", "dve", "pool", "sp")


class Prog:
    def __init__(self, nc, stack):
        self.nc = nc
        self.stack = stack
        self.ops = {e: [] for e in ENGS}
        self.esem = {}
        for e in ENGS:
            self.esem[e] = Sem(stack.enter_context(nc.semaphore("es_" + e)))
        self.dsems = []
        self.nsem = 0

    def sem(self):
        self.nsem += 1
        s = Sem(self.stack.enter_context(self.nc.semaphore("ds%d" % self.nsem)))
        self.dsems.append(s)
        return s

    def _record(self, eng, fn, r, w, pw, sig, dsem):
        op = Op()
        op.eng, op.fn, op.sig, op.dsem = eng, fn, sig, dsem
        op.idx = len(self.ops[eng])
        if dsem is not None:
            dsem.count += 16
            op.dval = dsem.count
            ev = ("d", dsem, op.dval)
            key = ("d", id(dsem))
        else:
            op.dval = 0
            ev = ("e", eng, op.idx)
            key = eng
        wd, rd = set(), set()
        for b in r:
            wd.update(b.writers)
        for b in w:
            wd.update(b.writers)
            rd.update(b.readers.values())
        for b in pw:
            rd.update(b.readers.values())
            rd.update(b.war)
        for b in list(r) + list(w) + list(pw):
            if b.excl:
                for e2, ev2 in b.last.items():
                    if e2 != eng:
                        wd.add(ev2)
                b.last[eng] = ev
        up = lambda evs: set((("d", e[1], e[1].count if e[1] is not dsem else e[2]) if e[0] == "d" else e) for e in evs)
        wd, rd = up(wd), up(rd)
        if dsem is not None:
            wd = set(e for e in wd if not (e[0] == "d" and e[1] is dsem and e[2] >= op.dval))
            rd = set(e for e in rd if not (e[0] == "d" and e[1] is dsem and e[2] >= op.dval))
        op.wdeps, op.rdeps = wd, rd
        for b in r:
            b.readers[key] = ev
        for b in w:
            b.war = set(b.readers.values())
            b.writers = [ev]
            b.readers = {}
        for b in pw:
            b.war = b.war | set(b.readers.values())
            b.writers.append(ev)
            b.readers = {}
        self.ops[eng].append(op)
        return op

    def op(self, eng, fn, r=(), w=(), pw=(), sig=True):
        return self._record(eng, fn, r, w, pw, sig, None)

    def dma(self, q, out, in_, sem, r=(), w=(), pw=()):
        return self._record(q, (out, in_), r, w, pw, True, sem)

    def barrier(self):
        evs = set()
        for e in ENGS:
            if self.ops[e]:
                last = self.ops[e][-1]
                if last.dsem is None and last.fn is not None:
                    last.sig = True
                evs.add(("e", e, len(self.ops[e]) - 1))
        for s in self.dsems:
            if s.count:
                evs.add(("d", s, s.count))
        for e in ENGS:
            op = Op()
            op.eng, op.fn, op.sig, op.dsem, op.dval = e, None, False, None, 0
            op.idx = len(self.ops[e])
            op.wdeps, op.rdeps = set(evs), set()
            self.ops[e].append(op)

    def emit(self, block):
        sigval = {}
        for e in ENGS:
            ops = self.ops[e]
            for op in reversed(ops):
                if op.fn is not None and op.dsem is None:
                    op.sig = True
                    break
            vals = [0] * len(ops)
            c = 0
            for i, op in enumerate(ops):
                if op.fn is not None and op.dsem is None and op.sig:
                    c += 1
                    vals[i] = c
            nxt = [0] * len(ops)
            cur = None
            for i in range(len(ops) - 1, -1, -1):
                if vals[i]:
                    cur = vals[i]
                nxt[i] = cur if cur is not None else c
            prev = 0
            for i, op in enumerate(ops):
                if op.fn is None or op.dsem is not None:
                    nxt[i] = prev
                elif vals[i]:
                    prev = vals[i]
            sigval[e] = (vals, nxt)

        def run(e, h):
            waited = {}
            vals, _ = sigval[e]
            mysem = self.esem[e]
            for i, op in enumerate(self.ops[e]):
                need = {}
                for kind, deps in (("w", op.wdeps), ("r", op.rdeps)):
                    for ev in deps:
                        if ev[0] == "e":
                            src, idx = ev[1], ev[2]
                            if src == e:
                                if e in ("pe", "sp") or kind == "r":
                                    continue
                                if op.fn is None:
                                    continue
                            v = sigval[src][1][idx]
                            s = self.esem[src]
                        else:
                            s, v = ev[1], ev[2]
                        if v <= 0:
                            continue
                        if need.get(s, 0) < v:
                            need[s] = v
                for s, v in need.items():
                    if waited.get(s, 0) >= v:
                        continue
                    h.wait_ge(s.h, v)
                    waited[s] = v
                if op.fn is None:
                    continue
                if op.dsem is not None:
                    out, in_ = op.fn
                    h.dma_start(out=out, in_=in_).then_inc(op.dsem.h, 16)
                else:
                    ins = op.fn(h)
                    if vals[i]:
                        ins.then_inc(mysem.h, 1)

        @block.tensor
        def _(h):
            run("pe", h)

        @block.scalar
        def _(h):
            run("act", h)

        @block.vector
        def _(h):
            run("dve", h)

        @block.gpsimd
        def _(h):
            run("pool", h)

        @block.sync
        def _(h):
            run("sp", h)


class _Stop(Exception):
    pass


def build_nc(dbg=None, stop_after=None):
    from contextlib import ExitStack
    nc = bass.Bass("TRN2", target_bir_lowering=False)

    SA = 99 if stop_after is None else stop_after

    def din(name, shape, dt=F32, ph=0):
        if ph > SA:
            return None
        return nc.dram_tensor(name, list(shape), dt, kind="ExternalInput").ap()

    def dscr(name, shape, dt):
        return nc.dram_tensor(name, list(shape), dt, kind="Internal").ap()

    x_ext = din("x_ext", [TE, D], ph=2)
    x_ctx = din("x_ctx", [1024, D], ph=0.5)
    c_t = din("c_t", [128, KC])
    ada_w = din("ada_w", [D, 6 * D])
    ada_b = din("ada_b", [1, 6 * D])
    n1g = din("n1g", [128, KC])
    n2g = din("n2g", [128, KC])
    w_in = din("w_in", [D, IN_W], ph=0.6)
    smalls = din("smalls", [128, 8])
    gsguT = din("gsguT", [128, 16])
    sgu_w = din("sgu_w", [8, 128, 128])
    sgu_bB = din("sgu_bB", [128, 8 * 128], ph=2)
    w_a = din("w_a", [2048, D], ph=4)
    w_b = din("w_b", [2048, D], ph=4)
    w_out = din("w_out", [D, D], ph=5)
    w_up = din("w_up", [D, 2 * DFF], ph=7)
    convw = din("convw", [128, 3 * 172])
    convb = din("convb", [128, 172])
    w_down = din("w_down", [DFF, D], ph=8)
    c_ident = din("c_ident", [128, 128])
    c_tril = din("c_tril", [128, 128])
    c_cmask = din("c_cmask", [128, 128])
    c_tmd = din("c_tmd", [128, 2048])
    out_d = nc.dram_tensor("out", [1024, D], F32, kind="ExternalOutput").ap()

    modrow = dscr("modrow", [1, 6 * D], F32)
    qT_d = dscr("qT_d", [16, 128, TE], BF16)
    qiT_d = dscr("qiT_d", [16, 128, TE], BF16)
    FT_d = dscr("FT_d", [9, 128, 16, 128], BF16)
    ybT_d = dscr("ybT_d", [16, 128, TE], BF16)
    sa_d = dscr("sa_d", [32, 128, TE], BF16)
    sb_d = dscr("sb_d", [32, 128, TE], BF16)
    xmid_d = dscr("xmid_d", [TE, D], F32)
    gat_d = dscr("gat_d", [8, 128, NFF, 128], BF16)

    dbg_outs = {}
    if dbg:
        for name, shape, dt in dbg:
            dbg_outs[name] = nc.dram_tensor("dbg_" + name, list(shape), dt, kind="ExternalOutput").ap()

    w_in_v = w_in.rearrange("(kc p) n -> p kc n", p=128) if w_in is not None else None
    ada_w_v = ada_w.rearrange("(kc p) n -> p kc n", p=128)
    w_a_v = w_a.rearrange("(kc p) n -> p kc n", p=128) if w_a is not None else None
    w_b_v = w_b.rearrange("(kc p) n -> p kc n", p=128) if w_b is not None else None
    w_out_v = w_out.rearrange("(kc p) n -> p kc n", p=128) if w_out is not None else None
    w_up_v = w_up.rearrange("(kc p) n -> p kc n", p=128) if w_up is not None else None
    w_down_v = w_down.rearrange("(kc p) n -> p kc n", p=128) if w_down is not None else None

    with ExitStack() as stack:
        arena = stack.enter_context(nc.sbuf_tensor("arena", [128, ARENA // 4], F32))
        psum = stack.enter_context(nc.psum_tensor("psum", [128, 4096], F32))
        P = Prog(nc, stack)

        def sb(off, shape, dt):
            esz = 4 if dt == F32 else 2
            n = 1
            for s in shape[1:]:
                n *= s
            nbytes = n * esz
            assert off % 4 == 0 and nbytes % 4 == 0 and off + nbytes <= ARENA, (off, shape)
            v = arena[0:shape[0], off // 4:(off + nbytes) // 4]
            if dt != F32:
                v = v.bitcast(dt)
            if len(shape) == 3:
                v = v.rearrange("p (a b) -> p a b", a=shape[1])
            elif len(shape) == 4:
                v = v.rearrange("p (a b c) -> p a b c", a=shape[1], b=shape[2])
            return v

        def bank(i, n=512):
            return psum[:, i * 512:i * 512 + n]

        def bank_bf(i):
            return psum[:, i * 512:(i + 1) * 512].bitcast(BF16)

        class Alloc:
            def __init__(self, base):
                self.off = base

            def get(self, shape, dt):
                esz = 4 if dt == F32 else 2
                n = 1
                for s in shape[1:]:
                    n *= s
                nb = (n * esz + 31) // 32 * 32
                v = sb(self.off, shape, dt)
                self.off += nb
                return v

        A0 = Alloc(0)
        ident = A0.get([128, 128], BF16)
        onesq = A0.get([128, 128], BF16)
        ones_f = A0.get([128, 128], F32)
        modT = A0.get([128, 192], F32)
        a1 = A0.get([128, KC], F32)
        a2 = A0.get([128, KC], F32)
        n1g_t = A0.get([128, KC], F32)
        n2g_t = A0.get([128, KC], F32)
        sm = A0.get([128, 8], F32)
        gsg = A0.get([128, 16], F32)
        cw_t = A0.get([128, 3 * 172], F32)
        cb_t = A0.get([128, 172], F32)
        cmask = A0.get([128, 128], F32)
        wabs = A0.get([128, 9, 32], F32)
        wsgn = A0.get([128, 9, 32], F32)
        tiny = A0.get([128, 64], F32)
        identf = A0.get([128, 128], F32)
        WsT = A0.get([128, 8, 128], BF16)
        PERS_END = A0.off
        assert PERS_END <= 12288, PERS_END
        KV0 = 12288
        AK = Alloc(KV0)
        kT = AK.get([128, 4, 2048], BF16)
        Vt = AK.get([128, 16, 512], BF16)
        kiT = AK.get([128, 2048], BF16)
        PH0 = AK.off
        gq, gk, ctxm, hflag, eps_t = sm[:, 0:1], sm[:, 1:2], sm[:, 2:3], sm[:, 3:4], sm[:, 4:5]

        B = {}

        def buf(name):
            if name not in B:
                B[name] = Buf(name)
            return B[name]

        bPS = [buf("ps%d" % i) for i in range(8)]
        for b_ in bPS:
            b_.excl = True

        DUMP = {}

        def ckpt(k):
            if SA == k:
                raise _Stop()

        try:
            s0 = P.sem()
            bconst = buf("const")
            tmp_f = sb(PH0, [128, 128], F32)
            tmp_f2 = sb(PH0 + 512, [128, 128], F32)
            for dst, src in ((tmp_f, c_ident), (n1g_t, n1g), (n2g_t, n2g), (sm, smalls), (gsg, gsguT),
                             (cw_t, convw), (cb_t, convb), (cmask, c_cmask)):
                P.dma("sp", dst, src, s0, pw=[bconst])
            P.op("dve", lambda h: h.tensor_copy(out=ident, in_=tmp_f), r=[bconst], pw=[bconst])
            P.op("dve", lambda h: h.tensor_copy(out=identf, in_=tmp_f), r=[bconst], pw=[bconst])
            P.op("dve", lambda h: h.memset(onesq, 1.0 / 128.0), pw=[bconst])
            P.op("dve", lambda h: h.memset(ones_f, 1.0), pw=[bconst])
            wsl = sb(PH0 + 1024, [128, 8, 128], F32)
            wsm = sb(PH0 + 1024 + 4096, [128, 8, 128], BF16)
            trilt = sb(PH0 + 1024 + 4096 + 2048, [128, 128], F32)
            bsg = buf("sguconst")
            s0b = P.sem()
            P.dma("sp", wsl, sgu_w.rearrange("g t s -> t g s"), s0b, pw=[bsg])
            P.dma("sp", trilt, c_tril, s0b, pw=[bsg])
            for g in range(8):
                P.op("dve", lambda h, g=g: h.tensor_tensor(out=wsm[:, g, :], in0=wsl[:, g, :], in1=trilt, op=ALU.mult), r=[bsg, bconst], pw=[bsg])
            for g in range(8):
                P.op("pe", lambda h, g=g: h.transpose(bank_bf(7)[:, g * 128:(g + 1) * 128], wsm[:, g, :], ident), r=[bsg, bconst],
                     w=[bPS[7]] if g == 0 else (), pw=[bPS[7]] if g else (), sig=(g == 7))
            P.op("dve", lambda h: h.tensor_copy(out=WsT, in_=bank_bf(7).rearrange("p (g t) -> p g t", g=8)), r=[bPS[7]], pw=[bsg])
            P.barrier()

            AP0 = Alloc(PH0)
            aw = [AP0.get([128, KC, 512], BF16) for _ in range(2)]
            abrow = [AP0.get([1, 512], F32) for _ in range(2)]
            rowsb = [AP0.get([1, 512], F32) for _ in range(2)]
            ct_f = AP0.get([128, KC], F32)
            cs_t = A0.get([128, KC], BF16)
            assert A0.off <= KV0, A0.off
            baw = [buf("aw0"), buf("aw1")]
            bab = [buf("ab0"), buf("ab1")]
            brow = [buf("row0"), buf("row1")]
            saw = [P.sem(), P.sem()]
            sab = [P.sem(), P.sem()]
            srow = [P.sem(), P.sem()]
            bcs = buf("cs")
            bmodT = buf("modT")
            P.dma("sp", ct_f, c_t, P.sem(), w=[bcs])
            P.op("act", lambda h: h.activation(out=cs_t, in_=ct_f, func=AF.Silu), r=[bcs], w=[bcs])
            NG0 = 16
            for g in range(NG0):
                sl = g % 2
                P.dma("pool", aw[sl], ada_w_v[:, :, g * 512:(g + 1) * 512], saw[sl], w=[baw[sl]])
                P.dma("sp", abrow[sl], ada_b[0:1, g * 512:(g + 1) * 512], sab[sl], w=[bab[sl]])
                pb = g % 2
                for kc in range(KC):
                    P.op("pe", lambda h, kc=kc, sl=sl, pb=pb: h.matmul(bank(pb)[0:1, :], lhsT=cs_t[:, kc:kc + 1], rhs=aw[sl][:, kc, :],
                                                                       start=(kc == 0), stop=(kc == KC - 1)),
                         r=[bcs, baw[sl]], w=[bPS[pb]] if kc == 0 else (), pw=[bPS[pb]] if kc else (), sig=(kc == KC - 1))
                P.op("dve", lambda h, sl=sl, pb=pb: h.tensor_tensor(out=rowsb[sl], in0=bank(pb)[0:1, :], in1=abrow[sl], op=ALU.add),
                     r=[bPS[pb], bab[sl]], w=[brow[sl]])
                P.dma("sp", modrow[0:1, g * 512:(g + 1) * 512], rowsb[sl], srow[sl], r=[brow[sl]], pw=[buf("modrow")])
                tb_ = 2 + (g % 2)
                for c in range(4):
                    P.op("pe", lambda h, c=c, sl=sl, tb_=tb_: h.matmul(bank(tb_)[:, c:c + 1], lhsT=rowsb[sl][0:1, c * 128:(c + 1) * 128],
                                                                      rhs=ones_f[0:1, 0:1], start=True, stop=True),
                         r=[brow[sl]], w=[bPS[tb_]] if c == 0 else (), pw=[bPS[tb_]] if c else (), sig=(c == 3))
                P.op("act", lambda h, g=g, tb_=tb_: h.activation(out=modT[:, g * 4:(g + 1) * 4], in_=bank(tb_)[:, 0:4], func=AF.Copy),
                     r=[bPS[tb_]], pw=[bmodT])
            sh1, sc1, sh2, sc2 = modT[:, 0:32], modT[:, 32:64], modT[:, 96:128], modT[:, 128:160]
            P.op("dve", lambda h: h.scalar_tensor_tensor(out=a1, in0=sc1, scalar=1.0, in1=n1g_t, op0=ALU.add, op1=ALU.mult),
                 r=[bmodT], pw=[bmodT])
            P.barrier()

            DUMP["modT"] = modT
            DUMP["a1"] = a1
            ckpt(0)
            HT0 = PH0
            hT = sb(HT0, [128, KC, TE], BF16)
            NA0 = HT0 + KC * TE * 2
            bhT = buf("hT")

            def norm_phase(src, blocks, a_t, sh_t, hT_view):
                AN = Alloc(NA0)
                xt = [AN.get([128, D], F32) for _ in range(2)]
                xn = [AN.get([128, D], BF16) for _ in range(2)]
                junk = AN.get([128, D], BF16)
                ss = AN.get([128, 4], F32)
                bxt = [buf("xt0"), buf("xt1")]
                bxn = [buf("xn0"), buf("xn1")]
                bss = [buf("ss0"), buf("ss1")]
                sx = [P.sem(), P.sem()]
                bjunk = buf("junk")
                for i, (r0, tlo, thi, d0) in enumerate(blocks):
                    sl = i % 2
                    P.dma("sp", xt[sl], src[r0:r0 + 128, :], sx[sl], w=[bxt[sl]])
                    ssv = ss[:, sl:sl + 1]
                    P.op("act", lambda h, sl=sl, ssv=ssv: h.activation(out=junk, in_=xt[sl], func=AF.Square, accum_out=ssv),
                         r=[bxt[sl]], w=[bss[sl], bjunk])
                    P.op("dve", lambda h, ssv=ssv: h.tensor_scalar(out=ssv, in0=ssv, scalar1=1.0 / D, scalar2=eps_t, op0=ALU.mult, op1=ALU.add),
                         r=[bss[sl]], w=[bss[sl]])
                    P.op("act", lambda h, ssv=ssv: h.activation(out=ssv, in_=ssv, func=AF.Sqrt), r=[bss[sl]], w=[bss[sl]])
                    P.op("dve", lambda h, ssv=ssv: h.reciprocal(out=ssv, in_=ssv), r=[bss[sl]], w=[bss[sl]])
                    P.op("dve", lambda h, sl=sl, ssv=ssv: h.tensor_scalar(out=xn[sl], in0=xt[sl], scalar1=ssv, scalar2=None, op0=ALU.mult),
                         r=[bss[sl], bxt[sl]], w=[bxn[sl]])
                    n = thi - tlo
                    if NORM_MODE == 1:
                        DUMP["xn"] = xn[sl]
                        DUMP["xt"] = xt[sl]
                        continue
                    for k8 in range(4):
                        pb = k8 % 2
                        for j in range(8):
                            kc = k8 * 8 + j
                            P.op("pe", lambda h, sl=sl, kc=kc, j=j, pb=pb: h.transpose(bank_bf(pb)[:, j * 128:(j + 1) * 128],
                                                                                     xn[sl][:, kc * 128:(kc + 1) * 128], ident),
                                 r=[bxn[sl]], w=[bPS[pb]] if j == 0 else (), pw=[bPS[pb]] if j else (), sig=(j == 7))
                        for j in range(8):
                            kc = k8 * 8 + j
                            dst = hT_view[:, kc, d0:d0 + n]
                            srcp = bank_bf(pb)[:, j * 128 + tlo:j * 128 + thi]
                            if NORM_MODE == 2:
                                P.op("dve", lambda h, dst=dst, srcp=srcp: h.tensor_copy(out=dst, in_=srcp), r=[bPS[pb], bmodT], pw=[bhT])
                            elif (j % 2 == 0 and NORM_MODE != 4) or NORM_MODE == 3:
                                P.op("act", lambda h, dst=dst, srcp=srcp, kc=kc: h.activation(out=dst, in_=srcp, func=AF.Identity,
                                                                                          scale=a_t[:, kc:kc + 1], bias=sh_t[:, kc:kc + 1]),
                                     r=[bPS[pb], bmodT], pw=[bhT])
                            else:
                                P.op("dve", lambda h, dst=dst, srcp=srcp, kc=kc: h.tensor_scalar(out=dst, in0=srcp, scalar1=a_t[:, kc:kc + 1],
                                                                                             scalar2=sh_t[:, kc:kc + 1], op0=ALU.mult, op1=ALU.add),
                                     r=[bPS[pb], bmodT], pw=[bhT])

            WS0 = NA0
            wslot = [sb(WS0 + i * 16384, [128, KC, 256], BF16) for i in range(2)]
            bws = [buf("ws0"), buf("ws1")]
            sws = [P.sem(), P.sem()]
            IP0 = WS0 + 2 * 16384
            wctr = [0]
            psrot = [0]

            def next_ps(nbanks=4):
                i = psrot[0] % nbanks
                psrot[0] += 1
                return i

            def load_w(wv, c0, n, dup64=False):
                sl = wctr[0] % 2
                wctr[0] += 1
                if dup64:
                    P.dma("pool", wslot[sl][:, :, 0:64], wv[:, :, c0:c0 + 64], sws[sl], w=[bws[sl]])
                    P.dma("pool", wslot[sl][:, :, 64:128], wv[:, :, c0:c0 + 64], sws[sl], pw=[bws[sl]])
                else:
                    P.dma("pool", wslot[sl][:, :, 0:n], wv[:, :, c0:c0 + n], sws[sl], w=[bws[sl]])
                return sl

            def fm_chunk(sl, coff, hTv, chunks, evac, rbufs):
                for tci, (t0, tl) in enumerate(chunks):
                    pb = next_ps()
                    for kc in range(KC):
                        P.op("pe", lambda h, kc=kc, pb=pb, t0=t0, tl=tl: h.matmul(bank(pb)[:, 0:tl], lhsT=wslot[sl][:, kc, coff:coff + 128],
                                                                                rhs=hTv[:, kc, t0:t0 + tl], start=(kc == 0), stop=(kc == KC - 1)),
                             r=[bws[sl]] + rbufs, w=[bPS[pb]] if kc == 0 else (), pw=[bPS[pb]] if kc else (), sig=(kc == KC - 1))
                    evac(tci, t0, tl, pb)

            def tm_block(sl, ncols, hTv, tok0, evac, rbufs):
                pb = next_ps()
                for kc in range(KC):
                    P.op("pe", lambda h, kc=kc, pb=pb: h.matmul(bank(pb)[:, 0:ncols], lhsT=hTv[:, kc, tok0:tok0 + 128],
                                                              rhs=wslot[sl][:, kc, 0:ncols], start=(kc == 0), stop=(kc == KC - 1)),
                         r=[bws[sl]] + rbufs, w=[bPS[pb]] if kc == 0 else (), pw=[bPS[pb]] if kc else (), sig=(kc == KC - 1))
                evac(pb)

            AI = Alloc(IP0)
            sqb = [AI.get([128, 344], BF16) for _ in range(2)]
            rsf = [AI.get([128, 344], F32) for _ in range(2)]
            stage = [AI.get([128, TE], BF16) for _ in range(2)]
            bsq = [buf("sq0"), buf("sq1")]
            brs = [buf("rs0"), buf("rs1")]
            bstage = [buf("stg0"), buf("stg1")]
            sstage = [P.sem(), P.sem()]
            qkctr = [0]
            stctr = [0]
            bkT, bV, bkiT = buf("kT"), buf("V"), buf("kiT")

            def qk_evac(pb, tl, gain, dst, dst_bufs, src_lo=0, wl=()):
                i = qkctr[0] % 2
                qkctr[0] += 1
                nb = 4 + i
                P.op("act", lambda h: h.activation(out=sqb[i][:, 0:tl], in_=bank(pb)[:, 0:tl], func=AF.Square),
                     r=[bPS[pb]], w=[bsq[i]])
                P.op("pe", lambda h: h.matmul(bank(nb)[:, 0:tl], lhsT=onesq, rhs=sqb[i][:, 0:tl], start=True, stop=True),
                     r=[bsq[i]], w=[bPS[nb]])
                P.op("act", lambda h: h.activation(out=rsf[i][:, 0:tl], in_=bank(nb)[:, 0:tl], func=AF.Sqrt, bias=eps_t, scale=1.0),
                     r=[bPS[nb]], w=[brs[i]])
                P.op("dve", lambda h: h.reciprocal(out=rsf[i][:, 0:tl], in_=rsf[i][:, 0:tl]), r=[brs[i]], w=[brs[i]])
                P.op("dve", lambda h: h.scalar_tensor_tensor(out=dst, in0=bank(pb)[:, src_lo:tl], scalar=gain, in1=rsf[i][:, src_lo:tl],
                                                             op0=ALU.mult, op1=ALU.mult),
                     r=[bPS[pb], brs[i]], w=list(wl), pw=list(dst_bufs))

            P.op("dve", lambda h: h.memset(stage[0], 0.0), w=[bstage[0]])
            P.op("dve", lambda h: h.memset(stage[1], 0.0), w=[bstage[1]])
            norm_phase(x_ctx, [(b * 128, 0, 128, b * 128) for b in range(8)], a1, sh1, hT)
            P.barrier()
            DUMP["hT"] = hT[:, :, 0:1024]
            ckpt(0.5)
            CTXC = [(0, 342), (342, 342), (684, 340)]
            for g in range(4):
                sl = load_w(w_in_v, C_K + g * 128, 128)

                def ev(tci, t0, tl, pb, g=g):
                    qk_evac(pb, tl, gk, kT[:, g, t0:t0 + tl], [bkT])
                fm_chunk(sl, 0, hT, CTXC, ev, [bhT])
            DUMP["kT"] = kT[:, :, 0:1024]
            ckpt(0.7)
            sl = load_w(w_in_v, C_KI, 64, dup64=True)

            def ev_ki_ctx(tci, t0, tl, pb):
                P.op("act", lambda h: h.activation(out=kiT[:, t0:t0 + tl], in_=bank(pb)[:, 0:tl], func=AF.Copy), r=[bPS[pb]], pw=[bkiT])
            fm_chunk(sl, 0, hT, CTXC, ev_ki_ctx, [bhT])
            DUMP["kiT"] = kiT[:, 0:1024]
            ckpt(0.8)
            for g2 in range(2):
                sl = load_w(w_in_v, C_V + g2 * 256, 256)
                for tb in range(8):
                    def ev_v(pb, tb=tb, g2=g2):
                        P.op("act", lambda h: h.activation(out=Vt[:, tb, g2 * 256:(g2 + 1) * 256], in_=bank(pb)[:, 0:256], func=AF.Copy),
                             r=[bPS[pb]], pw=[bV])
                    tm_block(sl, 256, hT, tb * 128, ev_v, [bhT])
            P.barrier()

            DUMP["kT"] = kT[:, :, 0:1024]
            DUMP["V"] = Vt[:, 0:8, :]
            DUMP["kiT"] = kiT[:, 0:1024]
            DUMP["hT"] = hT[:, :, 0:1024]
            ckpt(1)
            norm_phase(x_ext, [(b * 128, 0, 128, b * 128) for b in range(9)], a1, sh1, hT)
            P.barrier()

            def own_part(t0, tl):
                lo = max(t0, 128)
                return lo - t0, 1024 + (lo - 128)

            for g in range(4):
                sl = load_w(w_in_v, C_K + g * 128, 128)

                def ev(tci, t0, tl, pb, g=g):
                    lo, k0 = own_part(t0, tl)
                    qk_evac(pb, tl, gk, kT[:, g, k0:k0 + (tl - lo)], [bkT], src_lo=lo)
                fm_chunk(sl, 0, hT, FMC, ev, [bhT])
            sl = load_w(w_in_v, C_KI, 64, dup64=True)

            def ev_ki(tci, t0, tl, pb):
                lo, k0 = own_part(t0, tl)
                P.op("act", lambda h: h.activation(out=kiT[:, k0:k0 + (tl - lo)], in_=bank(pb)[:, lo:tl], func=AF.Copy), r=[bPS[pb]], pw=[bkiT])
            fm_chunk(sl, 0, hT, FMC, ev_ki, [bhT])
            for g2 in range(2):
                sl = load_w(w_in_v, C_V + g2 * 256, 256)
                for tb in range(8):
                    def ev_v(pb, tb=tb, g2=g2):
                        P.op("act", lambda h: h.activation(out=Vt[:, 8 + tb, g2 * 256:(g2 + 1) * 256], in_=bank(pb)[:, 0:256], func=AF.Copy),
                             r=[bPS[pb]], pw=[bV])
                    tm_block(sl, 256, hT, 128 + tb * 128, ev_v, [bhT])
            bwi = buf("wi")
            sl = load_w(w_in_v, C_WI, 32)
            for tb in range(9):
                def ev_wi(pb, tb=tb):
                    P.op("act", lambda h: h.activation(out=wabs[:, tb, :], in_=bank(pb)[:, 0:32], func=AF.Abs, scale=IDXS), r=[bPS[pb]], pw=[bwi])
                    P.op("act", lambda h: h.activation(out=wsgn[:, tb, :], in_=bank(pb)[:, 0:32], func=AF.Sign), r=[bPS[pb]], pw=[bwi])
                tm_block(sl, 32, hT, tb * 128, ev_wi, [bhT])

            VN0 = AI.off
            vn = AI.get([128, 9, 2048], BF16)
            bB = AI.get([128, 8, 128], F32)
            fstage = [AI.get([128, 16, 128], BF16)] * 2
            ssv_t = AI.get([128, 16], F32)
            assert AI.off <= ARENA, AI.off
            bvn = [buf("vn%d" % i) for i in range(9)]
            P.dma("sp", bB, sgu_bB.rearrange("p (g t) -> p g t", g=8), P.sem(), pw=[bsg])
            for g2 in range(8):
                sl = load_w(w_in_v, C_GV + g2 * 256, 256)
                for tb in range(9):
                    def ev_gv(pb, tb=tb, g2=g2):
                        P.op("act", lambda h: h.activation(out=vn[:, tb, g2 * 256:(g2 + 1) * 256], in_=bank(pb)[:, 0:256], func=AF.Gelu),
                             r=[bPS[pb]], pw=[bvn[tb]])
                    tm_block(sl, 256, hT, tb * 128, ev_gv, [bhT])
            sFT = [P.sem()] * 2
            bfst = [buf("fst0")] * 2
            bFT = buf("FT_d")
            for tb in range(9):
                ssv = ssv_t[:, tb:tb + 1]
                P.op("act", lambda h, tb=tb, ssv=ssv: h.activation(out=fstage[tb % 2].rearrange("p a b -> p (a b)"), in_=vn[:, tb, :],
                                                                  func=AF.Square, accum_out=ssv),
                     r=[bvn[tb]], w=[bfst[tb % 2], buf("ssv%d" % tb)])
                P.op("dve", lambda h, ssv=ssv: h.tensor_scalar(out=ssv, in0=ssv, scalar1=1.0 / 2048, scalar2=eps_t, op0=ALU.mult, op1=ALU.add),
                     r=[buf("ssv%d" % tb)], w=[buf("ssv%d" % tb)])
                P.op("act", lambda h, ssv=ssv: h.activation(out=ssv, in_=ssv, func=AF.Sqrt), r=[buf("ssv%d" % tb)], w=[buf("ssv%d" % tb)])
                P.op("dve", lambda h, ssv=ssv: h.reciprocal(out=ssv, in_=ssv), r=[buf("ssv%d" % tb)], w=[buf("ssv%d" % tb)])
                P.op("dve", lambda h, tb=tb, ssv=ssv: h.tensor_scalar(out=vn[:, tb, :], in0=vn[:, tb, :], scalar1=ssv, scalar2=None, op0=ALU.mult),
                     r=[buf("ssv%d" % tb), bvn[tb]], w=[bvn[tb]])
                fs = tb % 2
                for c in range(16):
                    P.op("pe", lambda h, tb=tb, c=c: h.matmul(bank(6)[:, (c % 4) * 128:(c % 4 + 1) * 128], lhsT=vn[:, tb, c * 128:(c + 1) * 128],
                                                            rhs=WsT[:, c // 2, :], start=True, stop=True),
                         r=[bvn[tb], bsg], w=[bPS[6]])
                    P.op("dve", lambda h, c=c, fs=fs: h.scalar_tensor_tensor(out=fstage[fs][:, c, :], in0=bank(6)[:, (c % 4) * 128:(c % 4 + 1) * 128],
                                                                           scalar=gsg[:, c:c + 1], in1=bB[:, c // 2, :], op0=ALU.mult, op1=ALU.add),
                         r=[bPS[6], bsg], w=[bfst[fs]] if c == 0 else (), pw=[bfst[fs]] if c else ())
                P.dma("sp", FT_d[tb], fstage[fs], sFT[fs], r=[bfst[fs]], pw=[bFT])

            ftc = [sb(VN0 + i * 2304, [128, 9, 128], BF16) for i in range(2)]
            gtmp = [sb(VN0 + 2 * 2304 + i * 1376, [128, 344], F32) for i in range(2)]
            bftc = [buf("ftc0"), buf("ftc1")]
            sftc = [P.sem(), P.sem()]
            bgt = [buf("gt0"), buf("gt1")]
            gctr = [0]
            for tb in range(9):
                pass
            P.barrier()

            M2 = VN0 + 2 * 2304 + 2 * 1376
            aw2 = [sb(M2 + i * 16384, [128, KC, 256], BF16) for i in range(2)]
            abrow2 = [sb(M2 + 32768 + i * 1024, [1, 256], F32) for i in range(2)]
            rowsb2 = [sb(M2 + 32768 + 2048 + i * 1024, [1, 256], F32) for i in range(2)]
            assert M2 + 32768 + 4096 <= ARENA
            baw2 = [buf("aw2_0"), buf("aw2_1")]
            bab2 = [buf("ab2_0"), buf("ab2_1")]
            brow2 = [buf("row2_0"), buf("row2_1")]
            saw2 = [P.sem(), P.sem()]
            sab2 = [P.sem(), P.sem()]
            srow2 = [P.sem(), P.sem()]
            modq = list(range(32, 96))

            def mod_step():
                if not modq:
                    return
                g = modq.pop(0)
                sl = g % 2
                c0_ = g * 256
                P.dma("pool", aw2[sl], ada_w_v[:, :, c0_:c0_ + 256], saw2[sl], w=[baw2[sl]])
                P.dma("sp", abrow2[sl], ada_b[0:1, c0_:c0_ + 256], sab2[sl], w=[bab2[sl]])
                for kc in range(KC):
                    P.op("pe", lambda h, kc=kc, sl=sl: h.matmul(bank(7)[0:1, 0:256], lhsT=cs_t[:, kc:kc + 1], rhs=aw2[sl][:, kc, :],
                                                               start=(kc == 0), stop=(kc == KC - 1)),
                         r=[bcs, baw2[sl]], w=[bPS[7]] if kc == 0 else (), pw=[bPS[7]] if kc else (), sig=(kc == KC - 1))
                P.op("dve", lambda h, sl=sl: h.tensor_tensor(out=rowsb2[sl], in0=bank(7)[0:1, 0:256], in1=abrow2[sl], op=ALU.add),
                     r=[bPS[7], bab2[sl]], w=[brow2[sl]])
                P.dma("sp", modrow[0:1, c0_:c0_ + 256], rowsb2[sl], srow2[sl], r=[brow2[sl]], pw=[buf("modrow")])
                for c in range(2):
                    P.op("pe", lambda h, c=c, sl=sl: h.matmul(bank(7)[:, 256 + c:257 + c], lhsT=rowsb2[sl][0:1, c * 128:(c + 1) * 128],
                                                             rhs=ones_f[0:1, 0:1], start=True, stop=True),
                         r=[brow2[sl]], w=[bPS[7]] if c == 0 else (), pw=[bPS[7]] if c else (), sig=(c == 1))
                P.op("act", lambda h, g=g: h.activation(out=modT[:, g * 2:(g + 1) * 2], in_=bank(7)[:, 256:258], func=AF.Copy),
                     r=[bPS[7]], pw=[bmodT])

            def spill_segment(c0, nchunks, dst_d, kind):
                ngr = (nchunks + 1) // 2
                for gi in range(ngr):
                    ncols = min(256, (nchunks - gi * 2) * 128)
                    sl = load_w(w_in_v, c0 + gi * 256, ncols)
                    for ci in range(ncols // 128):
                        ch = gi * 2 + ci
                        st = stctr[0] % 2
                        stctr[0] += 1
                        if kind == "gu":
                            fi = ch % 2
                            P.dma("sp", ftc[fi], FT_d[:, :, ch, :].rearrange("tb p t -> p tb t"), sftc[fi], r=[bFT], w=[bftc[fi]])

                        def ev(tci, t0, tl, pb, st=st, ch=ch, first=[True]):
                            dst = stage[st][:, t0:t0 + tl]
                            wl = [bstage[st]] if tci == 0 else []
                            pl = [bstage[st]] if tci else []
                            if kind == "q":
                                qk_evac(pb, tl, gq, dst, pl, src_lo=0, wl=wl)
                            elif kind == "qi":
                                if tci % 2 == 0:
                                    P.op("act", lambda h: h.activation(out=dst, in_=bank(pb)[:, 0:tl], func=AF.Copy), r=[bPS[pb]], w=wl, pw=pl)
                                else:
                                    P.op("dve", lambda h: h.tensor_copy(out=dst, in_=bank(pb)[:, 0:tl]), r=[bPS[pb]], w=wl, pw=pl)
                            elif kind == "sig":
                                P.op("act", lambda h: h.activation(out=dst, in_=bank(pb)[:, 0:tl], func=AF.Sigmoid), r=[bPS[pb]], w=wl, pw=pl)
                            elif kind == "gu":
                                gi_ = gctr[0] % 2
                                gctr[0] += 1
                                fi = ch % 2
                                P.op("act", lambda h: h.activation(out=gtmp[gi_][:, 0:tl], in_=bank(pb)[:, 0:tl], func=AF.Gelu),
                                     r=[bPS[pb]], w=[bgt[gi_]])
                                fsrc = ftc[fi].rearrange("p a b -> p (a b)")[:, t0:t0 + tl]
                                P.op("dve", lambda h: h.tensor_tensor(out=dst, in0=gtmp[gi_][:, 0:tl], in1=fsrc, op=ALU.mult),
                                     r=[bgt[gi_], bftc[fi]], w=wl, pw=pl)
                        fm_chunk(sl, ci * 128, hT, FMC, ev, [bhT])
                        P.dma("sp", dst_d[ch], stage[st], sstage[st], r=[bstage[st]], pw=[buf(kind + "_d")])
                        mod_step()

            spill_segment(C_GU, 16, ybT_d, "gu")
            byb = buf("gu_d")
            spill_segment(C_Q, 16, qT_d, "q")
            spill_segment(C_QI, 16, qiT_d, "qi")
            sig_d = buf("sig_d")
            spill_segment(C_GA, 32, sa_d, "sig")
            spill_segment(C_GB, 32, sb_d, "sig")
            while modq:
                mod_step()
            P.op("dve", lambda h: h.scalar_tensor_tensor(out=a2, in0=sc2, scalar=1.0, in1=n2g_t, op0=ALU.add, op1=ALU.mult),
                 r=[bmodT], pw=[bmodT])
            P.barrier()

            DUMP["wabs"] = wabs
            DUMP["wsgn"] = wsgn
            DUMP["qT_d"] = qT_d
            DUMP["qiT_d"] = qiT_d
            DUMP["ybT_d"] = ybT_d
            DUMP["sa_d"] = sa_d
            DUMP["sb_d"] = sb_d
            DUMP["FT_d"] = FT_d
            DUMP["kT"] = kT
            DUMP["V"] = Vt
            DUMP["kiT"] = kiT
            ckpt(2)
            AA = Alloc(PH0)
            qT = AA.get([128, 16, TE], BF16)
            yaT = AA.get([128, 16, TE], BF16)
            YAT_OFF = PH0 + 16 * TE * 2
            tmd = AA.get([128, 2048], F32)
            acc = AA.get([128, 2048], F32)
            work = AA.get([128, 2048], F32)
            distm = AA.get([128, 2048], F32)
            rbuf = [AA.get([128, 1024], BF16) for _ in range(2)]
            dg = AA.get([128, 32, 128], BF16)
            bdg = buf("dg")
            scb = [AA.get([128, 2048], F32) for _ in range(2)]
            Pb = [AA.get([128, 2048], BF16) for _ in range(2)]
            PT = [AA.get([128, 16, 128], BF16) for _ in range(2)]
            ya = AA.get([128, 2048], BF16)
            m8 = AA.get([128, 8], F32)
            thr = AA.get([128, 8], F32)
            stat = AA.get([128, 64], F32)
            assert AA.off <= ARENA, AA.off
            sq_ = P.sem()
            bq = buf("qT")
            bqi = [buf("qi%d" % i) for i in range(9)]
            for c in range(16):
                P.dma("sp", qT[:, c, :], qT_d[c], sq_, r=[buf("q_d")], pw=[bq])
                P.dma("sp", yaT[:, c, :], qiT_d[c], sq_, r=[buf("qi_d")], pw=bqi)
            P.dma("sp", tmd, c_tmd, sq_, w=[buf("tmd")])
            P.barrier()
            bacc, bwork, bdist, bthr = buf("acc"), buf("work"), buf("distm"), buf("thr")
            brb = [buf("rb0"), buf("rb1")]
            bsc = [buf("sc0"), buf("sc1")]
            bP = [buf("P0"), buf("P1")]
            bPT = [buf("PT0"), buf("PT1")]
            bya = buf("ya")
            bst = [buf("st0"), buf("st1")]
            rctr = [0]
            QB = [(0, 896.0, 1024, 7)] + [(128 + 128 * j, 1024.0 + 128 * j, 1024 + 128 * (j + 1), 8 + j) for j in range(8)]
            AB = [acc, distm]
            bAB = [bacc, bdist]

            def stage_A(qb):
                tok0, cst, Sk, dkb = QB[qb]
                accq, baccq = AB[qb % 2], bAB[qb % 2]
                for hh in range(32):
                    P.op("dve", lambda h, hh=hh, qb=qb: h.tensor_scalar(out=dg[:, hh, :], in0=ident, scalar1=wsgn[:, qb, hh:hh + 1], scalar2=None, op0=ALU.mult),
                         r=[bwi, bconst], w=[bdg] if hh == 0 else (), pw=[bdg] if hh else ())
                halves = [(0, 1024)] + ([(1024, Sk - 1024)] if Sk > 1024 else [])
                for (k0, kn) in halves:
                    nch = (kn + 511) // 512

                    def acc_mm(hh, ri, k0=k0, kn=kn):
                        for s0_ in range(0, kn, 512):
                            sn = min(512, kn - s0_)
                            ab = 4 + s0_ // 512
                            P.op("pe", lambda h, hh=hh, ri=ri, s0_=s0_, sn=sn, ab=ab: h.matmul(bank(ab)[:, 0:sn], lhsT=dg[:, hh, :], rhs=rbuf[ri][:, s0_:s0_ + sn],
                                                                                        start=(hh == 0), stop=(hh == 31)),
                                 r=[brb[ri], bdg], w=[bPS[ab]] if hh == 0 else (), pw=[bPS[ab]] if hh else (), sig=True)
                    prev = None
                    for hh in range(32):
                        c, hf = hh // 2, hh % 2
                        pbase = 2 * (rctr[0] % 2)
                        ri = rctr[0] % 2
                        rctr[0] += 1
                        lps = psum[:, pbase * 512:pbase * 512 + kn]
                        for s0_ in range(0, kn, 512):
                            sn = min(512, kn - s0_)
                            bi = pbase + s0_ // 512
                            P.op("pe", lambda h, c=c, hf=hf, s0_=s0_, sn=sn, bi=bi, k0=k0, tok0=tok0: h.matmul(
                                bank(bi)[:, 0:sn], lhsT=yaT[hf * 64:(hf + 1) * 64, c, tok0:tok0 + 128],
                                rhs=kiT[hf * 64:(hf + 1) * 64, k0 + s0_:k0 + s0_ + sn], start=True, stop=True),
                                r=[bqi[qb], bkiT], w=[bPS[bi]])
                        rbs = [bPS[pbase + i] for i in range(nch)]
                        P.op("act", lambda h, lps=lps, ri=ri, kn=kn, hh=hh, qb=qb: h.activation(out=rbuf[ri][:, 0:kn], in_=lps, func=AF.Relu,
                                                                                           scale=wabs[:, qb, hh:hh + 1]),
                             r=rbs + [bwi], w=[brb[ri]])
                        if prev is not None:
                            acc_mm(*prev)
                        prev = (hh, ri)
                    acc_mm(*prev)
                    aps = psum[:, 4 * 512:4 * 512 + kn]
                    abufs = [bPS[4 + i] for i in range(nch)]
                    if k0 == 0:
                        P.op("dve", lambda h, aps=aps, kn=kn: h.tensor_scalar(out=accq[:, 0:kn], in0=aps, scalar1=ctxm, scalar2=None, op0=ALU.add),
                             r=abufs, pw=[baccq])
                    else:
                        P.op("dve", lambda h, aps=aps, kn=kn, k0=k0: h.tensor_copy(out=accq[:, k0:k0 + kn], in_=aps), r=abufs, pw=[baccq])

            def stage_B(qb):
                tok0, cst, Sk, dkb = QB[qb]
                accq, baccq = AB[qb % 2], bAB[qb % 2]
                ops = []
                d0 = dkb * 128
                ops.append(lambda: P.op("dve", lambda h: h.tensor_tensor(out=accq[:, d0:d0 + 128], in0=accq[:, d0:d0 + 128], in1=cmask, op=ALU.add),
                                        r=[baccq], w=[baccq]))
                for it in range(32):
                    src = accq if it == 0 else work
                    ops.append(lambda src=src: P.op("dve", lambda h: h.max(out=m8, in_=src[:, 0:Sk]), r=[baccq, bwork], w=[bthr]))
                    if it < 31:
                        ops.append(lambda src=src: P.op("dve", lambda h: h.match_replace(out=work[:, 0:Sk], in_to_replace=m8, in_values=src[:, 0:Sk],
                                                                                      imm_value=-3.0e38), r=[bthr, baccq, bwork], w=[bwork]))
                ops.append(lambda: P.op("dve", lambda h: h.tensor_scalar(out=thr[:, 0:1], in0=m8[:, 7:8], scalar1=-1.0e29, scalar2=None, op0=ALU.max),
                                        r=[bthr], w=[bthr]))
                ops.append(lambda: P.op("dve", lambda h: h.tensor_scalar(out=work[:, 0:Sk], in0=accq[:, 0:Sk], scalar1=thr[:, 0:1], scalar2=-BIGD,
                                                                       op0=ALU.is_ge, op1=ALU.mult), r=[bthr, baccq], w=[bwork]))
                ops.append(lambda: P.op("dve", lambda h: h.scalar_tensor_tensor(out=accq[:, 0:Sk], in0=tmd[:, 0:Sk], scalar=cst + BIGD,
                                                                              in1=work[:, 0:Sk], op0=ALU.add, op1=ALU.add),
                                        r=[bwork, buf("tmd")], w=[baccq]))
                return ops

            def stage_C(qb, filler):
                tok0, cst, Sk, dkb = QB[qb]
                accq, baccq = AB[qb % 2], bAB[qb % 2]
                halves = [(0, 1024)] + ([(1024, Sk - 1024)] if Sk > 1024 else [])
                nkb = Sk // 128
                for hd in range(16):
                    g = hd // 4
                    si = hd % 2
                    for hi_, (k0, kn) in enumerate(halves):
                        pbase = 0 if hi_ == 0 else 2
                        for s0_ in range(0, kn, 512):
                            sn = min(512, kn - s0_)
                            bi = pbase + s0_ // 512
                            P.op("pe", lambda h, hd=hd, g=g, s0_=s0_, sn=sn, bi=bi, k0=k0, tok0=tok0: h.matmul(
                                bank(bi)[:, 0:sn], lhsT=qT[:, hd, tok0:tok0 + 128], rhs=kT[:, g, k0 + s0_:k0 + s0_ + sn],
                                start=True, stop=True), r=[bq, bkT], w=[bPS[bi]])
                        lps = psum[:, pbase * 512:pbase * 512 + kn]
                        rbs = [bPS[pbase + i] for i in range((kn + 511) // 512)]
                        P.op("dve", lambda h, si=si, k0=k0, kn=kn, lps=lps, hd=hd: h.scalar_tensor_tensor(
                            out=scb[si][:, k0:k0 + kn], in0=accq[:, k0:k0 + kn], scalar=-SLOPES[hd] / SCALE, in1=lps,
                            op0=ALU.mult, op1=ALU.add), r=rbs + [baccq], w=[bsc[si]] if hi_ == 0 else (), pw=[bsc[si]] if hi_ else ())
                    mx = stat[:, si * 4:si * 4 + 1]
                    nmx = stat[:, si * 4 + 1:si * 4 + 2]
                    rsum = stat[:, si * 4 + 2:si * 4 + 3]
                    P.op("dve", lambda h, si=si, Sk=Sk, mx=mx: h.reduce_max(out=mx, in_=scb[si][:, 0:Sk], axis=AX.X), r=[bsc[si]], w=[bst[si]])
                    P.op("dve", lambda h, mx=mx, nmx=nmx: h.tensor_scalar(out=nmx, in0=mx, scalar1=-SCALE, scalar2=None, op0=ALU.mult),
                         r=[bst[si]], w=[bst[si]])
                    P.op("act", lambda h, si=si, Sk=Sk, nmx=nmx, rsum=rsum: h.activation(out=Pb[si][:, 0:Sk], in_=scb[si][:, 0:Sk], func=AF.Exp,
                                                                                      bias=nmx, scale=SCALE, accum_out=rsum),
                         r=[bsc[si], bst[si]], w=[bP[si], buf("rsum%d" % si)])
                    for kb in range(nkb):
                        pb = 4 + kb // 8
                        P.op("pe", lambda h, si=si, kb=kb, pb=pb: h.transpose(bank_bf(pb)[:, (kb % 8) * 128:(kb % 8 + 1) * 128],
                                                                            Pb[si][:, kb * 128:(kb + 1) * 128], ident),
                             r=[bP[si]], w=[bPS[pb]] if kb % 8 == 0 else (), pw=[bPS[pb]] if kb % 8 else (),
                             sig=(kb % 8 == 7 or kb == nkb - 1))
                    for b2 in range((nkb + 7) // 8):
                        n8 = min(8, nkb - b2 * 8)
                        srcp = bank_bf(4 + b2)[:, 0:n8 * 128].rearrange("p (a b) -> p a b", a=n8)
                        if b2 == 0:
                            P.op("act", lambda h, si=si, b2=b2, n8=n8, srcp=srcp: h.activation(out=PT[si][:, b2 * 8:b2 * 8 + n8, :], in_=srcp, func=AF.Copy),
                                 r=[bPS[4 + b2]], w=[bPT[si]])
                        else:
                            P.op("dve", lambda h, si=si, b2=b2, n8=n8, srcp=srcp: h.tensor_copy(out=PT[si][:, b2 * 8:b2 * 8 + n8, :], in_=srcp),
                                 r=[bPS[4 + b2]], pw=[bPT[si]])
                    ob = 6 + hd % 2
                    for kb in range(nkb):
                        P.op("pe", lambda h, si=si, kb=kb, g=g, ob=ob, nkb=nkb: h.matmul(bank(ob)[:, 0:128], lhsT=PT[si][:, kb, :],
                                                                              rhs=Vt[:, kb, g * 128:(g + 1) * 128], start=(kb == 0), stop=(kb == nkb - 1)),
                             r=[bPT[si], bV], w=[bPS[ob]] if kb == 0 else (), pw=[bPS[ob]] if kb else (), sig=(kb == nkb - 1))
                    rinv = stat[:, si * 4 + 3:si * 4 + 4]
                    P.op("dve", lambda h, rsum=rsum, rinv=rinv: h.reciprocal(out=rinv, in_=rsum), r=[buf("rsum%d" % si)], w=[buf("rinv%d" % si)])
                    P.op("act", lambda h, hd=hd, ob=ob, rinv=rinv: h.activation(out=ya[:, hd * 128:(hd + 1) * 128], in_=bank(ob)[:, 0:128],
                                                                               func=AF.Copy, scale=rinv),
                         r=[bPS[ob], buf("rinv%d" % si)], pw=[bya])
                    filler(5)
                for b2 in range(2):
                    for j in range(8):
                        hd = b2 * 8 + j
                        P.op("pe", lambda h, hd=hd, j=j, b2=b2: h.transpose(bank_bf(4 + b2)[:, j * 128:(j + 1) * 128], ya[:, hd * 128:(hd + 1) * 128], ident),
                             r=[bya], w=[bPS[4 + b2]] if j == 0 else (), pw=[bPS[4 + b2]] if j else (), sig=(j == 7))
                    srcp = bank_bf(4 + b2).rearrange("p (a b) -> p a b", a=8)
                    P.op("dve", lambda h, b2=b2, tok0=tok0, srcp=srcp: h.tensor_copy(out=yaT[:, b2 * 8:(b2 + 1) * 8, tok0:tok0 + 128], in_=srcp),
                         r=[bPS[4 + b2]], w=[bqi[qb]] if b2 == 0 else (), pw=[bqi[qb]] if b2 else ())

            NQ = len(QB)
            stage_A(0)
            for t_ in stage_B(0):
                t_()
            if NQ > 1:
                stage_A(1)
            for qb in range(NQ):
                pend = stage_B(qb + 1) if qb + 1 < NQ else []

                def filler(n, pend=pend):
                    for _ in range(n):
                        if pend:
                            pend.pop(0)()
                stage_C(qb, filler)
                while pend:
                    pend.pop(0)()
                if qb + 2 < NQ:
                    stage_A(qb + 2)
            P.barrier()

            DUMP["yaT"] = yaT
            ckpt(3)
            AM = Alloc(KV0)
            mT = AM.get([128, KC, TE], BF16)
            assert AM.off <= YAT_OFF, (AM.off, YAT_OFF)
            AM2 = Alloc(YAT_OFF + 16 * TE * 2)
            ybT = AM2.get([128, 16, TE], BF16)
            wab = [[AM2.get([128, 16, 128], BF16) for _ in range(2)] for _ in range(2)]
            gst = [[AM2.get([128, TE], BF16) for _ in range(2)] for _ in range(2)]
            mtmp = [AM2.get([128, 344], F32) for _ in range(2)]
            assert AM2.off <= ARENA, AM2.off
            byb2 = buf("ybT")
            syb = P.sem()
            for c in range(16):
                P.dma("sp", ybT[:, c, :], ybT_d[c], syb, r=[buf("gu_d")], pw=[byb2])
            P.barrier()
            bwab = [[buf("wab%d%d" % (a, i)) for i in range(2)] for a in range(2)]
            swab = [[P.sem() for i in range(2)] for a in range(2)]
            bgst = [[buf("gst%d%d" % (a, i)) for i in range(2)] for a in range(2)]
            sgst = [[P.sem() for i in range(2)] for a in range(2)]
            bmt = [buf("mt0"), buf("mt1")]
            bmT = buf("mT")
            mctr = [0]
            for j in range(32):
                sl = j % 2
                P.dma("pool", wab[0][sl], w_a_v[:, :, j * 128:(j + 1) * 128], swab[0][sl], w=[bwab[0][sl]])
                P.dma("pool", wab[1][sl], w_b_v[:, :, j * 128:(j + 1) * 128], swab[1][sl], w=[bwab[1][sl]])
                P.dma("sp", gst[0][sl], sa_d[j], sgst[0][sl], r=[buf("sig_d")], w=[bgst[0][sl]])
                P.dma("sp", gst[1][sl], sb_d[j], sgst[1][sl], r=[buf("sig_d")], w=[bgst[1][sl]])
                for tci, (t0, tl) in enumerate(FMC):
                    pa = next_ps()
                    pbb = next_ps()
                    for kc in range(16):
                        P.op("pe", lambda h, kc=kc, pa=pa, t0=t0, tl=tl, sl=sl: h.matmul(bank(pa)[:, 0:tl], lhsT=wab[0][sl][:, kc, :], rhs=yaT[:, kc, t0:t0 + tl],
                                                                                     start=(kc == 0), stop=(kc == 15)),
                             r=[bwab[0][sl]] + bqi, w=[bPS[pa]] if kc == 0 else (), pw=[bPS[pa]] if kc else (), sig=(kc == 15))
                    for kc in range(16):
                        P.op("pe", lambda h, kc=kc, pbb=pbb, t0=t0, tl=tl, sl=sl: h.matmul(bank(pbb)[:, 0:tl], lhsT=wab[1][sl][:, kc, :], rhs=ybT[:, kc, t0:t0 + tl],
                                                                                       start=(kc == 0), stop=(kc == 15)),
                             r=[bwab[1][sl], byb2], w=[bPS[pbb]] if kc == 0 else (), pw=[bPS[pbb]] if kc else (), sig=(kc == 15))
                    mi = mctr[0] % 2
                    mctr[0] += 1
                    P.op("dve", lambda h, mi=mi, pa=pa, t0=t0, tl=tl, sl=sl: h.tensor_tensor(out=mtmp[mi][:, 0:tl], in0=bank(pa)[:, 0:tl], in1=gst[0][sl][:, t0:t0 + tl], op=ALU.mult),
                         r=[bPS[pa], bgst[0][sl]], w=[bmt[mi]])
                    P.op("dve", lambda h, mi=mi, pbb=pbb, t0=t0, tl=tl, sl=sl: h.tensor_tensor(out=bank(pbb)[:, 0:tl], in0=bank(pbb)[:, 0:tl], in1=gst[1][sl][:, t0:t0 + tl], op=ALU.mult),
                         r=[bPS[pbb], bgst[1][sl]], w=[bPS[pbb]])
                    P.op("dve", lambda h, mi=mi, pbb=pbb, t0=t0, tl=tl, j=j: h.tensor_tensor(out=mT[:, j, t0:t0 + tl], in0=bank(pbb)[:, 0:tl], in1=mtmp[mi][:, 0:tl], op=ALU.add),
                         r=[bPS[pbb], bmt[mi]], pw=[bmT])
            P.barrier()

            DUMP["mT"] = mT
            ckpt(4)
            AO = Alloc(KV0 + KC * TE * 2)
            wo = [AO.get([128, KC, 256], BF16) for _ in range(2)]
            g1b = AO.get([128, D], F32)
            xp = [AO.get([128, 256], F32) for _ in range(3)]
            assert AO.off <= ARENA
            bwo = [buf("wo0"), buf("wo1")]
            swo = [P.sem(), P.sem()]
            bxp = [buf("xp0"), buf("xp1"), buf("xp2")]
            sxp = [P.sem(), P.sem(), P.sem()]
            sxo = [P.sem(), P.sem(), P.sem()]
            bg1b = buf("g1b")
            P.dma("sp", g1b, modrow[0:1, 2 * D:3 * D].partition_broadcast(128), P.sem(), r=[buf("modrow")], w=[bg1b])
            bxm = buf("xmid_d")
            xctr = [0]
            for cg in range(16):
                sl = cg % 2
                P.dma("pool", wo[sl], w_out_v[:, :, cg * 256:(cg + 1) * 256], swo[sl], w=[bwo[sl]])
                for tb in range(9):
                    xi = xctr[0] % 3
                    xctr[0] += 1
                    P.dma("sp", xp[xi], x_ext[tb * 128:(tb + 1) * 128, cg * 256:(cg + 1) * 256], sxp[xi], w=[bxp[xi]])
                    pb = next_ps()
                    for kc in range(KC):
                        P.op("pe", lambda h, kc=kc, pb=pb, tb=tb, sl=sl: h.matmul(bank(pb)[:, 0:256], lhsT=mT[:, kc, tb * 128:(tb + 1) * 128], rhs=wo[sl][:, kc, :],
                                                                                start=(kc == 0), stop=(kc == KC - 1)),
                             r=[bmT, bwo[sl]], w=[bPS[pb]] if kc == 0 else (), pw=[bPS[pb]] if kc else (), sig=(kc == KC - 1))
                    P.op("dve", lambda h, pb=pb, cg=cg: h.tensor_tensor(out=bank(pb)[:, 0:256], in0=bank(pb)[:, 0:256], in1=g1b[:, cg * 256:(cg + 1) * 256], op=ALU.mult),
                         r=[bPS[pb], bg1b], w=[bPS[pb]])
                    P.op("dve", lambda h, pb=pb, xi=xi: h.tensor_tensor(out=xp[xi], in0=bank(pb)[:, 0:256], in1=xp[xi], op=ALU.add),
                         r=[bPS[pb], bxp[xi]], w=[bxp[xi]])
                    P.dma("act", xmid_d[tb * 128:(tb + 1) * 128, cg * 256:(cg + 1) * 256], xp[xi], sxo[xi], r=[bxp[xi]], pw=[bxm])
            P.barrier()

            DUMP["xmid_d"] = xmid_d
            ckpt(5)
            h2T = sb(HT0, [128, KC, 1026], BF16)
            xm_blocks = [(0, 126, 128, 0)] + [(128 + b * 128, 0, 128, 2 + b * 128) for b in range(8)]
            norm_phase(xmid_d, xm_blocks, a2, sh2, h2T)
            P.barrier()

            DUMP["h2T"] = h2T
            ckpt(6)
            H2END = HT0 + KC * 1026 * 2
            AU = Alloc((H2END + 31) // 32 * 32)
            wu = [AU.get([128, KC, 2, 128], BF16) for _ in range(2)]
            araw = [[AU.get([128, 1026], F32) for _ in range(2)] for _ in range(2)]
            cacc = [AU.get([128, 1024], F32) for _ in range(2)]
            sgl = AU.get([128, 1024], F32)
            gout = [AU.get([128, 1024], BF16) for _ in range(2)]
            assert AU.off <= ARENA, AU.off
            bwu = [buf("wu0"), buf("wu1")]
            swu = [P.sem(), P.sem()]
            bar = [[buf("ar%d%d" % (a, i)) for i in range(2)] for a in range(2)]
            bca = [buf("ca0"), buf("ca1")]
            bsgl = buf("sgl")
            bgo = [buf("go0"), buf("go1")]
            sgo = [P.sem(), P.sem()]
            bh2 = bhT
            UPC = [(0, 342), (342, 342), (684, 342)]
            bgat = buf("gat_d")
            for j in range(NFF):
                sl = j % 2
                P.dma("pool", wu[sl][:, :, 0, :], w_up_v[:, :, j * 128:(j + 1) * 128], swu[sl], w=[bwu[sl]])
                P.dma("pool", wu[sl][:, :, 1, :], w_up_v[:, :, DFF + j * 128:DFF + (j + 1) * 128], swu[sl], pw=[bwu[sl]])
                for gv_ in range(2):
                    ai = j % 2
                    for tci, (t0, tl) in enumerate(UPC):
                        pb = (gv_ * 3 + tci) if True else 0
                        for kc in range(KC):
                            P.op("pe", lambda h, kc=kc, pb=pb, t0=t0, tl=tl, sl=sl, gv_=gv_: h.matmul(bank(pb)[:, 0:tl], lhsT=wu[sl][:, kc, gv_, :], rhs=h2T[:, kc, t0:t0 + tl],
                                                                                              start=(kc == 0), stop=(kc == KC - 1)),
                                 r=[bwu[sl], bh2], w=[bPS[pb]] if kc == 0 else (), pw=[bPS[pb]] if kc else (), sig=(kc == KC - 1))
                        if tci % 2 == 0:
                            P.op("act", lambda h, pb=pb, t0=t0, tl=tl, gv_=gv_, ai=ai: h.activation(out=araw[gv_][ai][:, t0:t0 + tl], in_=bank(pb)[:, 0:tl], func=AF.Copy),
                                 r=[bPS[pb]], w=[bar[gv_][ai]] if tci == 0 else (), pw=[bar[gv_][ai]] if tci else ())
                        else:
                            P.op("dve", lambda h, pb=pb, t0=t0, tl=tl, gv_=gv_, ai=ai: h.tensor_copy(out=araw[gv_][ai][:, t0:t0 + tl], in_=bank(pb)[:, 0:tl]),
                                 r=[bPS[pb]], pw=[bar[gv_][ai]])
                    col = gv_ * NFF + j
                    ar = araw[gv_][ai]
                    P.op("dve", lambda h, ar=ar: h.tensor_scalar(out=ar[:, 0:2], in0=ar[:, 0:2], scalar1=hflag, scalar2=None, op0=ALU.mult),
                         r=[bar[gv_][ai]], w=[bar[gv_][ai]])
                    w0, w1, w2 = cw_t[:, col:col + 1], cw_t[:, 172 + col:172 + col + 1], cw_t[:, 344 + col:344 + col + 1]
                    cbv = cb_t[:, col:col + 1]
                    if gv_ == 0:
                        P.op("dve", lambda h, ar=ar, w2=w2, cbv=cbv, gv_=gv_: h.tensor_scalar(out=cacc[gv_], in0=ar[:, 2:1026], scalar1=w2, scalar2=cbv, op0=ALU.mult, op1=ALU.add),
                             r=[bar[gv_][ai]], w=[bca[gv_]])
                    else:
                        P.op("act", lambda h, ar=ar, w2=w2, cbv=cbv, gv_=gv_: h.activation(out=cacc[gv_], in_=ar[:, 2:1026], func=AF.Identity, scale=w2, bias=cbv),
                             r=[bar[gv_][ai]], w=[bca[gv_]])
                    P.op("dve", lambda h, ar=ar, w1=w1, gv_=gv_: h.scalar_tensor_tensor(out=cacc[gv_], in0=ar[:, 1:1025], scalar=w1, in1=cacc[gv_], op0=ALU.mult, op1=ALU.add),
                         r=[bar[gv_][ai], bca[gv_]], w=[bca[gv_]])
                    P.op("dve", lambda h, ar=ar, w0=w0, gv_=gv_: h.scalar_tensor_tensor(out=cacc[gv_], in0=ar[:, 0:1024], scalar=w0, in1=cacc[gv_], op0=ALU.mult, op1=ALU.add),
                         r=[bar[gv_][ai], bca[gv_]], w=[bca[gv_]])
                P.op("act", lambda h: h.activation(out=sgl, in_=cacc[0], func=AF.Silu), r=[bca[0]], w=[bsgl])
                gi = j % 2
                P.op("dve", lambda h, gi=gi: h.tensor_tensor(out=gout[gi], in0=sgl, in1=cacc[1], op=ALU.mult), r=[bsgl, bca[1]], w=[bgo[gi]])
                P.dma("sp", gat_d[:, :, j, :].rearrange("tb p t -> p tb t"), gout[gi].rearrange("p (tb t) -> p tb t", tb=8), sgo[gi], r=[bgo[gi]], pw=[bgat])
            P.barrier()

            DUMP["gat_d"] = gat_d
            ckpt(7)
            AD = Alloc(KV0)
            wd = [AD.get([128, 43, 512], BF16) for _ in range(2)]
            gp = [AD.get([128, 43, 128], BF16) for _ in range(3)]
            g2b = AD.get([128, D], F32)
            xo = [AD.get([128, 512], F32) for _ in range(3)]
            assert AD.off <= ARENA, AD.off
            bwd = [buf("wd0"), buf("wd1")]
            swd = [P.sem(), P.sem()]
            bgp = [buf("gp0"), buf("gp1"), buf("gp2")]
            sgp = [P.sem(), P.sem(), P.sem()]
            bxo = [buf("xo0"), buf("xo1"), buf("xo2")]
            sxi = [P.sem(), P.sem(), P.sem()]
            sxw = [P.sem(), P.sem(), P.sem()]
            bg2b = buf("g2b")
            P.dma("sp", g2b, modrow[0:1, 5 * D:6 * D].partition_broadcast(128), P.sem(), r=[buf("modrow")], w=[bg2b])
            gctr2 = [0]
            octr = [0]
            bout = buf("out")
            for cg in range(8):
                for hf in range(2):
                    P.dma("pool", wd[hf], w_down_v[:, hf * 43:(hf + 1) * 43, cg * 512:(cg + 1) * 512], swd[hf], w=[bwd[hf]])
                    for tb in range(8):
                        gi = gctr2[0] % 3
                        gctr2[0] += 1
                        P.dma("sp", gp[gi], gat_d[tb, :, hf * 43:(hf + 1) * 43, :], sgp[gi], r=[bgat], w=[bgp[gi]])
                        for kc in range(43):
                            P.op("pe", lambda h, kc=kc, tb=tb, hf=hf, gi=gi: h.matmul(bank(tb), lhsT=gp[gi][:, kc, :], rhs=wd[hf][:, kc, :],
                                                                                    start=(hf == 0 and kc == 0), stop=(hf == 1 and kc == 42)),
                                 r=[bgp[gi], bwd[hf]], w=[bPS[tb]] if (hf == 0 and kc == 0) else (), pw=() if (hf == 0 and kc == 0) else [bPS[tb]],
                                 sig=(kc == 42))
                        if hf == 1:
                            oi = octr[0] % 3
                            octr[0] += 1
                            P.dma("sp", xo[oi], xmid_d[128 + tb * 128:128 + (tb + 1) * 128, cg * 512:(cg + 1) * 512], sxi[oi], r=[bxm], w=[bxo[oi]])
                            P.op("dve", lambda h, tb=tb, cg=cg: h.tensor_tensor(out=bank(tb), in0=bank(tb), in1=g2b[:, cg * 512:(cg + 1) * 512], op=ALU.mult),
                                 r=[bPS[tb], bg2b], w=[bPS[tb]])
                            P.op("dve", lambda h, tb=tb, oi=oi: h.tensor_tensor(out=xo[oi], in0=bank(tb), in1=xo[oi], op=ALU.add),
                                 r=[bPS[tb], bxo[oi]], w=[bxo[oi]])
                            P.dma("act", out_d[tb * 128:(tb + 1) * 128, cg * 512:(cg + 1) * 512], xo[oi], sxw[oi], r=[bxo[oi]], pw=[bout])
            P.barrier()

        except _Stop:
            pass
        if dbg:
            P.barrier()
            sdb = P.sem()
            for name, shape, dt in dbg:
                src = DUMP[name]
                P.dma("sp", dbg_outs[name], src, sdb)
            P.barrier()
        if stop_after is not None:
            sfin = P.sem()
            P.dma("sp", out_d[0:128, 0:128], ones_f, sfin)
            P.barrier()
        print("ops:", {e: len(P.ops[e]) for e in ENGS}, "dma sems:", P.nsem, flush=True)
        with nc.Block() as block:
            P.emit(block)
    return nc


dbg_qb = [8]
NORM_MODE = 0


def _consts():
    t = np.arange(128)[:, None]
    s = np.arange(128)[None, :]
    ident = np.eye(128, dtype=np.float32)
    tril = (s <= t).astype(np.float32)
    cmask = np.where(s <= t, 0.0, NEG).astype(np.float32)
    tmd = (np.arange(128)[:, None] - np.arange(2048)[None, :]).astype(np.float32)
    return ident, tril, cmask, tmd


_NC_CACHE = {}


def make_in_maps(x, c, ada_w, ada_b, norm1_g, w_in, q_norm_g, k_norm_g, sgu_norm_g, sgu_w, sgu_b,
                 w_branch_a, w_branch_b, w_out, norm2_g, w_up, conv_w, conv_b, w_down):
    f = lambda a: np.ascontiguousarray(np.asarray(a, dtype=np.float32))
    x = f(x); c = f(c)
    ident, tril, cmask, tmd = _consts()
    shared = {
        "ada_w": f(ada_w[0]), "ada_b": f(ada_b[0]).reshape(1, -1),
        "n1g": f(np.asarray(norm1_g[0]).reshape(KC, 128).T), "n2g": f(np.asarray(norm2_g[0]).reshape(KC, 128).T),
        "w_in": f(w_in[0]),
        "gsguT": f(np.asarray(sgu_norm_g[0]).reshape(16, 128).T),
        "sgu_w": f(sgu_w[0]),
        "sgu_bB": f(np.broadcast_to(np.asarray(sgu_b[0]).reshape(1, 1024), (128, 1024))),
        "w_a": f(w_branch_a[0]), "w_b": f(w_branch_b[0]), "w_out": f(w_out[0]), "w_up": f(w_up[0]),
        "convw": f(np.asarray(conv_w[0]).reshape(3, 172, 128).transpose(2, 0, 1).reshape(128, 3 * 172)),
        "convb": f(np.asarray(conv_b[0]).reshape(172, 128).T),
        "w_down": f(w_down[0]),
        "c_ident": ident, "c_tril": tril, "c_cmask": cmask, "c_tmd": tmd,
    }
    gq = np.asarray(q_norm_g[0], dtype=np.float32)
    gk = np.asarray(k_norm_g[0], dtype=np.float32)
    in_maps = []
    for core in range(8):
        b, hf = core // 2, core % 2
        own = x[b, hf * 1024:(hf + 1) * 1024]
        if hf == 1:
            ctx = x[b, 0:1024]
        else:
            ctx = np.zeros((1024, D), np.float32)
        x_ext = np.ascontiguousarray(np.concatenate([ctx[896:1024], own], axis=0))
        sm = np.zeros((128, 8), np.float32)
        sm[:, 0] = gq
        sm[:, 1] = gk
        sm[:, 2] = 0.0 if hf == 1 else NEG
        sm[:, 3] = 1.0 if hf == 1 else 0.0
        sm[:, 4] = EPS
        m = dict(shared)
        m["x_ext"] = x_ext
        m["x_ctx"] = np.ascontiguousarray(ctx)
        m["c_t"] = f(c[b].reshape(KC, 128).T)
        m["smalls"] = sm
        in_maps.append(m)
    return in_maps


PHASE_INPUTS = {"x_ctx": 0.5, "w_in": 0.6, "x_ext": 2, "sgu_bB": 2, "w_a": 4, "w_b": 4, "w_out": 5, "w_up": 7, "w_down": 8}


def filter_inputs(in_maps, stop_after):
    if stop_after is None:
        return in_maps
    return [{k: v for k, v in m.items() if PHASE_INPUTS.get(k, 0) <= stop_after} for m in in_maps]


def kernel(**inputs):
    if "nc" not in _NC_CACHE:
        _NC_CACHE["nc"] = build_nc()
    nc = _NC_CACHE["nc"]
    in_maps = make_in_maps(**inputs)
    res = run_bass_kernel_spmd(nc, in_maps, core_ids=list(range(8)))
    out = np.empty((NB, SEQ, D), np.float32)
    for core in range(8):
        b, hf = core // 2, core % 2
        out[b, hf * 1024:(hf + 1) * 1024] = res.results[core]["out"]
    return out
```

```python
import numpy as np
import concourse.bass as bass
import concourse.mybir as mybir
from concourse.bass_utils import run_bass_kernel_spmd

F32 = mybir.dt.float32
BF16 = mybir.dt.bfloat16
AF = mybir.ActivationFunctionType
ALU = mybir.AluOpType
AX = mybir.AxisListType

D = 4096
KC = 32
SEQ = 2048
NB = 4
IN_W = 17504
DFF = 11008
NFF = 86
TE = 1152
FMC = [(126, 342), (468, 342), (810, 342)]
C_Q, C_K, C_V, C_QI, C_KI, C_WI, C_GU, C_GV, C_GA, C_GB = 0, 2048, 2560, 3072, 5120, 5184, 5216, 7264, 9312, 13408
EPS = 1e-6
NEG = -1.0e30
BIGD = 1.0e7
SCALE = 128.0 ** -0.5
IDXS = (64.0 ** -0.5) * (32.0 ** -0.5)
SLOPES = [2.0 ** (-8.0 * (i + 1) / 16) for i in range(16)]
ARENA = 212000


class Buf:
    __slots__ = ("name", "writers", "readers", "war", "excl", "last")

    def __init__(self, name, excl=False):
        self.name = name
        self.writers = []
        self.readers = {}
        self.war = set()
        self.excl = excl
        self.last = {}


class Sem:
    def __init__(self, h):
        self.h = h
        self.count = 0


class Op:
    __slots__ = ("eng", "fn", "wdeps", "rdeps", "sig", "idx", "dsem", "dval")


ENGS = ("pe", "act", "dve", "pool", "sp")


class Prog:
    def __init__(self, nc, stack):
        self.nc = nc
        self.stack = stack
        self.ops = {e: [] for e in ENGS}
        self.esem = {}
        for e in ENGS:
            self.esem[e] = Sem(stack.enter_context(nc.semaphore("es_" + e)))
        self.dsems = []
        self.nsem = 0

    def sem(self):
        self.nsem += 1
        s = Sem(self.stack.enter_context(self.nc.semaphore("ds%d" % self.nsem)))
        self.dsems.append(s)
        return s

    def _record(self, eng, fn, r, w, pw, sig, dsem):
        op = Op()
        op.eng, op.fn, op.sig, op.dsem = eng, fn, sig, dsem
        op.idx = len(self.ops[eng])
        if dsem is not None:
            dsem.count += 16
            op.dval = dsem.count
            ev = ("d", dsem, op.dval)
            key = ("d", id(dsem))
        else:
            op.dval = 0
            ev = ("e", eng, op.idx)
            key = eng
        wd, rd = set(), set()
        for b in r:
            wd.update(b.writers)
        for b in w:
            wd.update(b.writers)
            rd.update(b.readers.values())
        for b in pw:
            rd.update(b.readers.values())
            rd.update(b.war)
        for b in list(r) + list(w) + list(pw):
            if b.excl:
                for e2, ev2 in b.last.items():
                    if e2 != eng:
                        wd.add(ev2)
                b.last[eng] = ev
        up = lambda evs: set((("d", e[1], e[1].count if e[1] is not dsem else e[2]) if e[0] == "d" else e) for e in evs)
        wd, rd = up(wd), up(rd)
        if dsem is not None:
            wd = set(e for e in wd if not (e[0] == "d" and e[1] is dsem and e[2] >= op.dval))
            rd = set(e for e in rd if not (e[0] == "d" and e[1] is dsem and e[2] >= op.dval))
        op.wdeps, op.rdeps = wd, rd
        for b in r:
            b.readers[key] = ev
        for b in w:
            b.war = set(b.readers.values())
            b.writers = [ev]
            b.readers = {}
        for b in pw:
            b.war = b.war | set(b.readers.values())
            b.writers.append(ev)
            b.readers = {}
        self.ops[eng].append(op)
        return op

    def op(self, eng, fn, r=(), w=(), pw=(), sig=True):
        return self._record(eng, fn, r, w, pw, sig, None)

    def dma(self, q, out, in_, sem, r=(), w=(), pw=()):
        return self._record(q, (out, in_), r, w, pw, True, sem)

    def barrier(self):
        evs = set()
        for e in ENGS:
            if self.ops[e]:
                last = self.ops[e][-1]
                if last.dsem is None and last.fn is not None:
                    last.sig = True
                evs.add(("e", e, len(self.ops[e]) - 1))
        for s in self.dsems:
            if s.count:
                evs.add(("d", s, s.count))
        for e in ENGS:
            op = Op()
            op.eng, op.fn, op.sig, op.dsem, op.dval = e, None, False, None, 0
            op.idx = len(self.ops[e])
            op.wdeps, op.rdeps = set(evs), set()
            self.ops[e].append(op)

    def emit(self, block):
        sigval = {}
        for e in ENGS:
            ops = self.ops[e]
            for op in reversed(ops):
                if op.fn is not None and op.dsem is None:
                    op.sig = True
                    break
            vals = [0] * len(ops)
            c = 0
            for i, op in enumerate(ops):
                if op.fn is not None and op.dsem is None and op.sig:
                    c += 1
                    vals[i] = c
            nxt = [0] * len(ops)
            cur = None
            for i in range(len(ops) - 1, -1, -1):
                if vals[i]:
                    cur = vals[i]
                nxt[i] = cur if cur is not None else c
            prev = 0
            for i, op in enumerate(ops):
                if op.fn is None or op.dsem is not None:
                    nxt[i] = prev
                elif vals[i]:
                    prev = vals[i]
            sigval[e] = (vals, nxt)

        def run(e, h):
            waited = {}
            vals, _ = sigval[e]
            mysem = self.esem[e]
            for i, op in enumerate(self.ops[e]):
                need = {}
                for kind, deps in (("w", op.wdeps), ("r", op.rdeps)):
                    for ev in deps:
                        if ev[0] == "e":
                            src, idx = ev[1], ev[2]
                            if src == e:
                                if e in ("pe", "sp") or kind == "r":
                                    continue
                                if op.fn is None:
                                    continue
                            v = sigval[src][1][idx]
                            s = self.esem[src]
                        else:
                            s, v = ev[1], ev[2]
                        if v <= 0:
                            continue
                        if need.get(s, 0) < v:
                            need[s] = v
                for s, v in need.items():
                    if waited.get(s, 0) >= v:
                        continue
                    h.wait_ge(s.h, v)
                    waited[s] = v
                if op.fn is None:
                    continue
                if op.dsem is not None:
                    out, in_ = op.fn
                    h.dma_start(out=out, in_=in_).then_inc(op.dsem.h, 16)
                else:
                    ins = op.fn(h)
                    if vals[i]:
                        ins.then_inc(mysem.h, 1)

        @block.tensor
        def _(h):
            run("pe", h)

        @block.scalar
        def _(h):
            run("act", h)

        @block.vector
        def _(h):
            run("dve", h)

        @block.gpsimd
        def _(h):
            run("pool", h)

        @block.sync
        def _(h):
            run("sp", h)


class _Stop(Exception):
    pass


def build_nc(dbg=None, stop_after=None):
    from contextlib import ExitStack
    nc = bass.Bass("TRN2", target_bir_lowering=False)

    SA = 99 if stop_after is None else stop_after

    def din(name, shape, dt=F32, ph=0):
        if ph > SA:
            return None
        return nc.dram_tensor(name, list(shape), dt, kind="ExternalInput").ap()

    def dscr(name, shape, dt):
        return nc.dram_tensor(name, list(shape), dt, kind="Internal").ap()

    x_ext = din("x_ext", [TE, D], ph=2)
    x_ctx = din("x_ctx", [1024, D], ph=0.5)
    c_t = din("c_t", [128, KC])
    ada_w = din("ada_w", [D, 6 * D])
    ada_b = din("ada_b", [1, 6 * D])
    n1g = din("n1g", [128, KC])
    n2g = din("n2g", [128, KC])
    w_in = din("w_in", [D, IN_W], ph=0.6)
    smalls = din("smalls", [128, 8])
    gsguT = din("gsguT", [128, 16])
    sgu_w = din("sgu_w", [8, 128, 128])
    sgu_bB = din("sgu_bB", [128, 8 * 128], ph=2)
    w_a = din("w_a", [2048, D], ph=4)
    w_b = din("w_b", [2048, D], ph=4)
    w_out = din("w_out", [D, D], ph=5)
    w_up = din("w_up", [D, 2 * DFF], ph=7)
    convw = din("convw", [128, 3 * 172])
    convb = din("convb", [128, 172])
    w_down = din("w_down", [DFF, D], ph=8)
    c_ident = din("c_ident", [128, 128])
    c_tril = din("c_tril", [128, 128])
    c_cmask = din("c_cmask", [128, 128])
    c_tmd = din("c_tmd", [128, 2048])
    out_d = nc.dram_tensor("out", [1024, D], F32, kind="ExternalOutput").ap()

    modrow = dscr("modrow", [1, 6 * D], F32)
    qT_d = dscr("qT_d", [16, 128, TE], BF16)
    qiT_d = dscr("qiT_d", [16, 128, TE], BF16)
    FT_d = dscr("FT_d", [9, 128, 16, 128], BF16)
    ybT_d = dscr("ybT_d", [16, 128, TE], BF16)
    sa_d = dscr("sa_d", [32, 128, TE], BF16)
    sb_d = dscr("sb_d", [32, 128, TE], BF16)
    xmid_d = dscr("xmid_d", [TE, D], F32)
    gat_d = dscr("gat_d", [8, 128, NFF, 128], BF16)

    dbg_outs = {}
    if dbg:
        for name, shape, dt in dbg:
            dbg_outs[name] = nc.dram_tensor("dbg_" + name, list(shape), dt, kind="ExternalOutput").ap()

    w_in_v = w_in.rearrange("(kc p) n -> p kc n", p=128) if w_in is not None else None
    ada_w_v = ada_w.rearrange("(kc p) n -> p kc n", p=128)
    w_a_v = w_a.rearrange("(kc p) n -> p kc n", p=128) if w_a is not None else None
    w_b_v = w_b.rearrange("(kc p) n -> p kc n", p=128) if w_b is not None else None
    w_out_v = w_out.rearrange("(kc p) n -> p kc n", p=128) if w_out is not None else None
    w_up_v = w_up.rearrange("(kc p) n -> p kc n", p=128) if w_up is not None else None
    w_down_v = w_down.rearrange("(kc p) n -> p kc n", p=128) if w_down is not None else None

    with ExitStack() as stack:
        arena = stack.enter_context(nc.sbuf_tensor("arena", [128, ARENA // 4], F32))
        psum = stack.enter_context(nc.psum_tensor("psum", [128, 4096], F32))
        P = Prog(nc, stack)

        def sb(off, shape, dt):
            esz = 4 if dt == F32 else 2
            n = 1
            for s in shape[1:]:
                n *= s
            nbytes = n * esz
            assert off % 4 == 0 and nbytes % 4 == 0 and off + nbytes <= ARENA, (off, shape)
            v = arena[0:shape[0], off // 4:(off + nbytes) // 4]
            if dt != F32:
                v = v.bitcast(dt)
            if len(shape) == 3:
                v = v.rearrange("p (a b) -> p a b", a=shape[1])
            elif len(shape) == 4:
                v = v.rearrange("p (a b c) -> p a b c", a=shape[1], b=shape[2])
            return v

        def bank(i, n=512):
            return psum[:, i * 512:i * 512 + n]

        def bank_bf(i):
            return psum[:, i * 512:(i + 1) * 512].bitcast(BF16)

        class Alloc:
            def __init__(self, base):
                self.off = base

            def get(self, shape, dt):
                esz = 4 if dt == F32 else 2
                n = 1
                for s in shape[1:]:
                    n *= s
                nb = (n * esz + 31) // 32 * 32
                v = sb(self.off, shape, dt)
                self.off += nb
                return v

        A0 = Alloc(0)
        ident = A0.get([128, 128], BF16)
        onesq = A0.get([128, 128], BF16)
        ones_f = A0.get([128, 128], F32)
        modT = A0.get([128, 192], F32)
        a1 = A0.get([128, KC], F32)
        a2 = A0.get([128, KC], F32)
        n1g_t = A0.get([128, KC], F32)
        n2g_t = A0.get([128, KC], F32)
        sm = A0.get([128, 8], F32)
        gsg = A0.get([128, 16], F32)
        cw_t = A0.get([128, 3 * 172], F32)
        cb_t = A0.get([128, 172], F32)
        cmask = A0.get([128, 128], F32)
        wabs = A0.get([128, 9, 32], F32)
        wsgn = A0.get([128, 9, 32], F32)
        tiny = A0.get([128, 64], F32)
        identf = A0.get([128, 128], F32)
        WsT = A0.get([128, 8, 128], BF16)
        PERS_END = A0.off
        assert PERS_END <= 12288, PERS_END
        KV0 = 12288
        AK = Alloc(KV0)
        kT = AK.get([128, 4, 2048], BF16)
        Vt = AK.get([128, 16, 512], BF16)
        kiT = AK.get([128, 2048], BF16)
        PH0 = AK.off
        gq, gk, ctxm, hflag, eps_t = sm[:, 0:1], sm[:, 1:2], sm[:, 2:3], sm[:, 3:4], sm[:, 4:5]

        B = {}

        def buf(name):
            if name not in B:
                B[name] = Buf(name)
            return B[name]

        bPS = [buf("ps%d" % i) for i in range(8)]
        for b_ in bPS:
            b_.excl = True

        DUMP = {}

        def ckpt(k):
            if SA == k:
                raise _Stop()

        try:
            s0 = P.sem()
            bconst = buf("const")
            tmp_f = sb(PH0, [128, 128], F32)
            tmp_f2 = sb(PH0 + 512, [128, 128], F32)
            for dst, src in ((tmp_f, c_ident), (n1g_t, n1g), (n2g_t, n2g), (sm, smalls), (gsg, gsguT),
                             (cw_t, convw), (cb_t, convb), (cmask, c_cmask)):
                P.dma("sp", dst, src, s0, pw=[bconst])
            P.op("dve", lambda h: h.tensor_copy(out=ident, in_=tmp_f), r=[bconst], pw=[bconst])
            P.op("dve", lambda h: h.tensor_copy(out=identf, in_=tmp_f), r=[bconst], pw=[bconst])
            P.op("dve", lambda h: h.memset(onesq, 1.0 / 128.0), pw=[bconst])
            P.op("dve", lambda h: h.memset(ones_f, 1.0), pw=[bconst])
            wsl = sb(PH0 + 1024, [128, 8, 128], F32)
            wsm = sb(PH0 + 1024 + 4096, [128, 8, 128], BF16)
            trilt = sb(PH0 + 1024 + 4096 + 2048, [128, 128], F32)
            bsg = buf("sguconst")
            s0b = P.sem()
            P.dma("sp", wsl, sgu_w.rearrange("g t s -> t g s"), s0b, pw=[bsg])
            P.dma("sp", trilt, c_tril, s0b, pw=[bsg])
            for g in range(8):
                P.op("dve", lambda h, g=g: h.tensor_tensor(out=wsm[:, g, :], in0=wsl[:, g, :], in1=trilt, op=ALU.mult), r=[bsg, bconst], pw=[bsg])
            for g in range(8):
                P.op("pe", lambda h, g=g: h.transpose(bank_bf(7)[:, g * 128:(g + 1) * 128], wsm[:, g, :], ident), r=[bsg, bconst],
                     w=[bPS[7]] if g == 0 else (), pw=[bPS[7]] if g else (), sig=(g == 7))
            P.op("dve", lambda h: h.tensor_copy(out=WsT, in_=bank_bf(7).rearrange("p (g t) -> p g t", g=8)), r=[bPS[7]], pw=[bsg])
            P.barrier()

            AP0 = Alloc(PH0)
            aw = [AP0.get([128, KC, 512], BF16) for _ in range(2)]
            abrow = [AP0.get([1, 512], F32) for _ in range(2)]
            rowsb = [AP0.get([1, 512], F32) for _ in range(2)]
            ct_f = AP0.get([128, KC], F32)
            cs_t = A0.get([128, KC], BF16)
            assert A0.off <= KV0, A0.off
            baw = [buf("aw0"), buf("aw1")]
            bab = [buf("ab0"), buf("ab1")]
            brow = [buf("row0"), buf("row1")]
            saw = [P.sem(), P.sem()]
            sab = [P.sem(), P.sem()]
            srow = [P.sem(), P.sem()]
            bcs = buf("cs")
            bmodT = buf("modT")
            P.dma("sp", ct_f, c_t, P.sem(), w=[bcs])
            P.op("act", lambda h: h.activation(out=cs_t, in_=ct_f, func=AF.Silu), r=[bcs], w=[bcs])
            NG0 = 16
            for g in range(NG0):
                sl = g % 2
                P.dma("pool", aw[sl], ada_w_v[:, :, g * 512:(g + 1) * 512], saw[sl], w=[baw[sl]])
                P.dma("sp", abrow[sl], ada_b[0:1, g * 512:(g + 1) * 512], sab[sl], w=[bab[sl]])
                pb = g % 2
                for kc in range(KC):
                    P.op("pe", lambda h, kc=kc, sl=sl, pb=pb: h.matmul(bank(pb)[0:1, :], lhsT=cs_t[:, kc:kc + 1], rhs=aw[sl][:, kc, :],
                                                                       start=(kc == 0), stop=(kc == KC - 1)),
                         r=[bcs, baw[sl]], w=[bPS[pb]] if kc == 0 else (), pw=[bPS[pb]] if kc else (), sig=(kc == KC - 1))
                P.op("dve", lambda h, sl=sl, pb=pb: h.tensor_tensor(out=rowsb[sl], in0=bank(pb)[0:1, :], in1=abrow[sl], op=ALU.add),
                     r=[bPS[pb], bab[sl]], w=[brow[sl]])
                P.dma("sp", modrow[0:1, g * 512:(g + 1) * 512], rowsb[sl], srow[sl], r=[brow[sl]], pw=[buf("modrow")])
                tb_ = 2 + (g % 2)
                for c in range(4):
                    P.op("pe", lambda h, c=c, sl=sl, tb_=tb_: h.matmul(bank(tb_)[:, c:c + 1], lhsT=rowsb[sl][0:1, c * 128:(c + 1) * 128],
                                                                      rhs=ones_f[0:1, 0:1], start=True, stop=True),
                         r=[brow[sl]], w=[bPS[tb_]] if c == 0 else (), pw=[bPS[tb_]] if c else (), sig=(c == 3))
                P.op("act", lambda h, g=g, tb_=tb_: h.activation(out=modT[:, g * 4:(g + 1) * 4], in_=bank(tb_)[:, 0:4], func=AF.Copy),
                     r=[bPS[tb_]], pw=[bmodT])
            sh1, sc1, sh2, sc2 = modT[:, 0:32], modT[:, 32:64], modT[:, 96:128], modT[:, 128:160]
            P.op("dve", lambda h: h.scalar_tensor_tensor(out=a1, in0=sc1, scalar=1.0, in1=n1g_t, op0=ALU.add, op1=ALU.mult),
                 r=[bmodT], pw=[bmodT])
            P.barrier()

            DUMP["modT"] = modT
            DUMP["a1"] = a1
            ckpt(0)
            HT0 = PH0
            hT = sb(HT0, [128, KC, TE], BF16)
            NA0 = HT0 + KC * TE * 2
            bhT = buf("hT")

            def norm_phase(src, blocks, a_t, sh_t, hT_view):
                AN = Alloc(NA0)
                xt = [AN.get([128, D], F32) for _ in range(2)]
                xn = [AN.get([128, D], BF16) for _ in range(2)]
                junk = AN.get([128, D], BF16)
                ss = AN.get([128, 4], F32)
                bxt = [buf("xt0"), buf("xt1")]
                bxn = [buf("xn0"), buf("xn1")]
                bss = [buf("ss0"), buf("ss1")]
                sx = [P.sem(), P.sem()]
                bjunk = buf("junk")
                for i, (r0, tlo, thi, d0) in enumerate(blocks):
                    sl = i % 2
                    P.dma("sp", xt[sl], src[r0:r0 + 128, :], sx[sl], w=[bxt[sl]])
                    ssv = ss[:, sl:sl + 1]
                    P.op("act", lambda h, sl=sl, ssv=ssv: h.activation(out=junk, in_=xt[sl], func=AF.Square, accum_out=ssv),
                         r=[bxt[sl]], w=[bss[sl], bjunk])
                    P.op("dve", lambda h, ssv=ssv: h.tensor_scalar(out=ssv, in0=ssv, scalar1=1.0 / D, scalar2=eps_t, op0=ALU.mult, op1=ALU.add),
                         r=[bss[sl]], w=[bss[sl]])
                    P.op("act", lambda h, ssv=ssv: h.activation(out=ssv, in_=ssv, func=AF.Sqrt), r=[bss[sl]], w=[bss[sl]])
                    P.op("dve", lambda h, ssv=ssv: h.reciprocal(out=ssv, in_=ssv), r=[bss[sl]], w=[bss[sl]])
                    P.op("dve", lambda h, sl=sl, ssv=ssv: h.tensor_scalar(out=xn[sl], in0=xt[sl], scalar1=ssv, scalar2=None, op0=ALU.mult),
                         r=[bss[sl], bxt[sl]], w=[bxn[sl]])
                    n = thi - tlo
                    if NORM_MODE == 1:
                        DUMP["xn"] = xn[sl]
                        DUMP["xt"] = xt[sl]
                        continue
                    for k8 in range(4):
                        pb = k8 % 2
                        for j in range(8):
                            kc = k8 * 8 + j
                            P.op("pe", lambda h, sl=sl, kc=kc, j=j, pb=pb: h.transpose(bank_bf(pb)[:, j * 128:(j + 1) * 128],
                                                                                     xn[sl][:, kc * 128:(kc + 1) * 128], ident),
                                 r=[bxn[sl]], w=[bPS[pb]] if j == 0 else (), pw=[bPS[pb]] if j else (), sig=(j == 7))
                        for j in range(8):
                            kc = k8 * 8 + j
                            dst = hT_view[:, kc, d0:d0 + n]
                            srcp = bank_bf(pb)[:, j * 128 + tlo:j * 128 + thi]
                            if NORM_MODE == 2:
                                P.op("dve", lambda h, dst=dst, srcp=srcp: h.tensor_copy(out=dst, in_=srcp), r=[bPS[pb], bmodT], pw=[bhT])
                            elif (j % 2 == 0 and NORM_MODE != 4) or NORM_MODE == 3:
                                P.op("act", lambda h, dst=dst, srcp=srcp, kc=kc: h.activation(out=dst, in_=srcp, func=AF.Identity,
                                                                                          scale=a_t[:, kc:kc + 1], bias=sh_t[:, kc:kc + 1]),
                                     r=[bPS[pb], bmodT], pw=[bhT])
                            else:
                                P.op("dve", lambda h, dst=dst, srcp=srcp, kc=kc: h.tensor_scalar(out=dst, in0=srcp, scalar1=a_t[:, kc:kc + 1],
                                                                                             scalar2=sh_t[:, kc:kc + 1], op0=ALU.mult, op1=ALU.add),
                                     r=[bPS[pb], bmodT], pw=[bhT])

            WS0 = NA0
            wslot = [sb(WS0 + i * 16384, [128, KC, 256], BF16) for i in range(2)]
            bws = [buf("ws0"), buf("ws1")]
            sws = [P.sem(), P.sem()]
            IP0 = WS0 + 2 * 16384
            wctr = [0]
            psrot = [0]

            def next_ps(nbanks=4):
                i = psrot[0] % nbanks
                psrot[0] += 1
                return i

            def load_w(wv, c0, n, dup64=False):
                sl = wctr[0] % 2
                wctr[0] += 1
                if dup64:
                    P.dma("pool", wslot[sl][:, :, 0:64], wv[:, :, c0:c0 + 64], sws[sl], w=[bws[sl]])
                    P.dma("pool", wslot[sl][:, :, 64:128], wv[:, :, c0:c0 + 64], sws[sl], pw=[bws[sl]])
                else:
                    P.dma("pool", wslot[sl][:, :, 0:n], wv[:, :, c0:c0 + n], sws[sl], w=[bws[sl]])
                return sl

            def fm_chunk(sl, coff, hTv, chunks, evac, rbufs):
                for tci, (t0, tl) in enumerate(chunks):
                    pb = next_ps()
                    for kc in range(KC):
                        P.op("pe", lambda h, kc=kc, pb=pb, t0=t0, tl=tl: h.matmul(bank(pb)[:, 0:tl], lhsT=wslot[sl][:, kc, coff:coff + 128],
                                                                                rhs=hTv[:, kc, t0:t0 + tl], start=(kc == 0), stop=(kc == KC - 1)),
                             r=[bws[sl]] + rbufs, w=[bPS[pb]] if kc == 0 else (), pw=[bPS[pb]] if kc else (), sig=(kc == KC - 1))
                    evac(tci, t0, tl, pb)

            def tm_block(sl, ncols, hTv, tok0, evac, rbufs):
                pb = next_ps()
                for kc in range(KC):
                    P.op("pe", lambda h, kc=kc, pb=pb: h.matmul(bank(pb)[:, 0:ncols], lhsT=hTv[:, kc, tok0:tok0 + 128],
                                                              rhs=wslot[sl][:, kc, 0:ncols], start=(kc == 0), stop=(kc == KC - 1)),
                         r=[bws[sl]] + rbufs, w=[bPS[pb]] if kc == 0 else (), pw=[bPS[pb]] if kc else (), sig=(kc == KC - 1))
                evac(pb)

            AI = Alloc(IP0)
            sqb = [AI.get([128, 344], BF16) for _ in range(2)]
            rsf = [AI.get([128, 344], F32) for _ in range(2)]
            stage = [AI.get([128, TE], BF16) for _ in range(2)]
            bsq = [buf("sq0"), buf("sq1")]
            brs = [buf("rs0"), buf("rs1")]
            bstage = [buf("stg0"), buf("stg1")]
            sstage = [P.sem(), P.sem()]
            qkctr = [0]
            stctr = [0]
            bkT, bV, bkiT = buf("kT"), buf("V"), buf("kiT")

            def qk_evac(pb, tl, gain, dst, dst_bufs, src_lo=0, wl=()):
                i = qkctr[0] % 2
                qkctr[0] += 1
                nb = 4 + i
                P.op("act", lambda h: h.activation(out=sqb[i][:, 0:tl], in_=bank(pb)[:, 0:tl], func=AF.Square),
                     r=[bPS[pb]], w=[bsq[i]])
                P.op("pe", lambda h: h.matmul(bank(nb)[:, 0:tl], lhsT=onesq, rhs=sqb[i][:, 0:tl], start=True, stop=True),
                     r=[bsq[i]], w=[bPS[nb]])
                P.op("act", lambda h: h.activation(out=rsf[i][:, 0:tl], in_=bank(nb)[:, 0:tl], func=AF.Sqrt, bias=eps_t, scale=1.0),
                     r=[bPS[nb]], w=[brs[i]])
                P.op("dve", lambda h: h.reciprocal(out=rsf[i][:, 0:tl], in_=rsf[i][:, 0:tl]), r=[brs[i]], w=[brs[i]])
                P.op("dve", lambda h: h.scalar_tensor_tensor(out=dst, in0=bank(pb)[:, src_lo:tl], scalar=gain, in1=rsf[i][:, src_lo:tl],
                                                             op0=ALU.mult, op1=ALU.mult),
                     r=[bPS[pb], brs[i]], w=list(wl), pw=list(dst_bufs))

            P.op("dve", lambda h: h.memset(stage[0], 0.0), w=[bstage[0]])
            P.op("dve", lambda h: h.memset(stage[1], 0.0), w=[bstage[1]])
            norm_phase(x_ctx, [(b * 128, 0, 128, b * 128) for b in range(8)], a1, sh1, hT)
            P.barrier()
            DUMP["hT"] = hT[:, :, 0:1024]
            ckpt(0.5)
            CTXC = [(0, 342), (342, 342), (684, 340)]
            for g in range(4):
                sl = load_w(w_in_v, C_K + g * 128, 128)

                def ev(tci, t0, tl, pb, g=g):
                    qk_evac(pb, tl, gk, kT[:, g, t0:t0 + tl], [bkT])
                fm_chunk(sl, 0, hT, CTXC, ev, [bhT])
            DUMP["kT"] = kT[:, :, 0:1024]
            ckpt(0.7)
            sl = load_w(w_in_v, C_KI, 64, dup64=True)

            def ev_ki_ctx(tci, t0, tl, pb):
                P.op("act", lambda h: h.activation(out=kiT[:, t0:t0 + tl], in_=bank(pb)[:, 0:tl], func=AF.Copy), r=[bPS[pb]], pw=[bkiT])
            fm_chunk(sl, 0, hT, CTXC, ev_ki_ctx, [bhT])
            DUMP["kiT"] = kiT[:, 0:1024]
            ckpt(0.8)
            for g2 in range(2):
                sl = load_w(w_in_v, C_V + g2 * 256, 256)
                for tb in range(8):
                    def ev_v(pb, tb=tb, g2=g2):
                        P.op("act", lambda h: h.activation(out=Vt[:, tb, g2 * 256:(g2 + 1) * 256], in_=bank(pb)[:, 0:256], func=AF.Copy),
                             r=[bPS[pb]], pw=[bV])
                    tm_block(sl, 256, hT, tb * 128, ev_v, [bhT])
            P.barrier()

            DUMP["kT"] = kT[:, :, 0:1024]
            DUMP["V"] = Vt[:, 0:8, :]
            DUMP["kiT"] = kiT[:, 0:1024]
            DUMP["hT"] = hT[:, :, 0:1024]
            ckpt(1)
            norm_phase(x_ext, [(b * 128, 0, 128, b * 128) for b in range(9)], a1, sh1, hT)
            P.barrier()

            def own_part(t0, tl):
                lo = max(t0, 128)
                return lo - t0, 1024 + (lo - 128)

            for g in range(4):
                sl = load_w(w_in_v, C_K + g * 128, 128)

                def ev(tci, t0, tl, pb, g=g):
                    lo, k0 = own_part(t0, tl)
                    qk_evac(pb, tl, gk, kT[:, g, k0:k0 + (tl - lo)], [bkT], src_lo=lo)
                fm_chunk(sl, 0, hT, FMC, ev, [bhT])
            sl = load_w(w_in_v, C_KI, 64, dup64=True)

            def ev_ki(tci, t0, tl, pb):
                lo, k0 = own_part(t0, tl)
                P.op("act", lambda h: h.activation(out=kiT[:, k0:k0 + (tl - lo)], in_=bank(pb)[:, lo:tl], func=AF.Copy), r=[bPS[pb]], pw=[bkiT])
            fm_chunk(sl, 0, hT, FMC, ev_ki, [bhT])
            for g2 in range(2):
                sl = load_w(w_in_v, C_V + g2 * 256, 256)
                for tb in range(8):
                    def ev_v(pb, tb=tb, g2=g2):
                        P.op("act", lambda h: h.activation(out=Vt[:, 8 + tb, g2 * 256:(g2 + 1) * 256], in_=bank(pb)[:, 0:256], func=AF.Copy),
                             r=[bPS[pb]], pw=[bV])
                    tm_block(sl, 256, hT, 128 + tb * 128, ev_v, [bhT])
            bwi = buf("wi")
            sl = load_w(w_in_v, C_WI, 32)
            for tb in range(9):
                def ev_wi(pb, tb=tb):
                    P.op("act", lambda h: h.activation(out=wabs[:, tb, :], in_=bank(pb)[:, 0:32], func=AF.Abs, scale=IDXS), r=[bPS[pb]], pw=[bwi])
                    P.op("act", lambda h: h.activation(out=wsgn[:, tb, :], in_=bank(pb)[:, 0:32], func=AF.Sign), r=[bPS[pb]], pw=[bwi])
                tm_block(sl, 32, hT, tb * 128, ev_wi, [bhT])

            VN0 = AI.off
            vn = AI.get([128, 9, 2048], BF16)
            bB = AI.get([128, 8, 128], F32)
            fstage = [AI.get([128, 16, 128], BF16)] * 2
            ssv_t = AI.get([128, 16], F32)
            assert AI.off <= ARENA, AI.off
            bvn = [buf("vn%d" % i) for i in range(9)]
            P.dma("sp", bB, sgu_bB.rearrange("p (g t) -> p g t", g=8), P.sem(), pw=[bsg])
            for g2 in range(8):
                sl = load_w(w_in_v, C_GV + g2 * 256, 256)
                for tb in range(9):
                    def ev_gv(pb, tb=tb, g2=g2):
                        P.op("act", lambda h: h.activation(out=vn[:, tb, g2 * 256:(g2 + 1) * 256], in_=bank(pb)[:, 0:256], func=AF.Gelu),
                             r=[bPS[pb]], pw=[bvn[tb]])
                    tm_block(sl, 256, hT, tb * 128, ev_gv, [bhT])
            sFT = [P.sem()] * 2
            bfst = [buf("fst0")] * 2
            bFT = buf("FT_d")
            for tb in range(9):
                ssv = ssv_t[:, tb:tb + 1]
                P.op("act", lambda h, tb=tb, ssv=ssv: h.activation(out=fstage[tb % 2].rearrange("p a b -> p (a b)"), in_=vn[:, tb, :],
                                                                  func=AF.Square, accum_out=ssv),
                     r=[bvn[tb]], w=[bfst[tb % 2], buf("ssv%d" % tb)])
                P.op("dve", lambda h, ssv=ssv: h.tensor_scalar(out=ssv, in0=ssv, scalar1=1.0 / 2048, scalar2=eps_t, op0=ALU.mult, op1=ALU.add),
                     r=[buf("ssv%d" % tb)], w=[buf("ssv%d" % tb)])
                P.op("act", lambda h, ssv=ssv: h.activation(out=ssv, in_=ssv, func=AF.Sqrt), r=[buf("ssv%d" % tb)], w=[buf("ssv%d" % tb)])
                P.op("dve", lambda h, ssv=ssv: h.reciprocal(out=ssv, in_=ssv), r=[buf("ssv%d" % tb)], w=[buf("ssv%d" % tb)])
                P.op("dve", lambda h, tb=tb, ssv=ssv: h.tensor_scalar(out=vn[:, tb, :], in0=vn[:, tb, :], scalar1=ssv, scalar2=None, op0=ALU.mult),
                     r=[buf("ssv%d" % tb), bvn[tb]], w=[bvn[tb]])
                fs = tb % 2
                for c in range(16):
                    P.op("pe", lambda h, tb=tb, c=c: h.matmul(bank(6)[:, (c % 4) * 128:(c % 4 + 1) * 128], lhsT=vn[:, tb, c * 128:(c + 1) * 128],
                                                            rhs=WsT[:, c // 2, :], start=True, stop=True),
                         r=[bvn[tb], bsg], w=[bPS[6]])
                    P.op("dve", lambda h, c=c, fs=fs: h.scalar_tensor_tensor(out=fstage[fs][:, c, :], in0=bank(6)[:, (c % 4) * 128:(c % 4 + 1) * 128],
                                                                           scalar=gsg[:, c:c + 1], in1=bB[:, c // 2, :], op0=ALU.mult, op1=ALU.add),
                         r=[bPS[6], bsg], w=[bfst[fs]] if c == 0 else (), pw=[bfst[fs]] if c else ())
                P.dma("sp", FT_d[tb], fstage[fs], sFT[fs], r=[bfst[fs]], pw=[bFT])

            ftc = [sb(VN0 + i * 2304, [128, 9, 128], BF16) for i in range(2)]
            gtmp = [sb(VN0 + 2 * 2304 + i * 1376, [128, 344], F32) for i in range(2)]
            bftc = [buf("ftc0"), buf("ftc1")]
            sftc = [P.sem(), P.sem()]
            bgt = [buf("gt0"), buf("gt1")]
            gctr = [0]
            for tb in range(9):
                pass
            P.barrier()

            M2 = VN0 + 2 * 2304 + 2 * 1376
            aw2 = [sb(M2 + i * 16384, [128, KC, 256], BF16) for i in range(2)]
            abrow2 = [sb(M2 + 32768 + i * 1024, [1, 256], F32) for i in range(2)]
            rowsb2 = [sb(M2 + 32768 + 2048 + i * 1024, [1, 256], F32) for i in range(2)]
            assert M2 + 32768 + 4096 <= ARENA
            baw2 = [buf("aw2_0"), buf("aw2_1")]
            bab2 = [buf("ab2_0"), buf("ab2_1")]
            brow2 = [buf("row2_0"), buf("row2_1")]
            saw2 = [P.sem(), P.sem()]
            sab2 = [P.sem(), P.sem()]
            srow2 = [P.sem(), P.sem()]
            modq = []

            def mod_step():
                if not modq:
                    return
                g = modq.pop(0)
                sl = g % 2
                c0_ = g * 256
                P.dma("pool", aw2[sl], ada_w_v[:, :, c0_:c0_ + 256], saw2[sl], w=[baw2[sl]])
                P.dma("sp", abrow2[sl], ada_b[0:1, c0_:c0_ + 256], sab2[sl], w=[bab2[sl]])
                for kc in range(KC):
                    P.op("pe", lambda h, kc=kc, sl=sl: h.matmul(bank(7)[0:1, 0:256], lhsT=cs_t[:, kc:kc + 1], rhs=aw2[sl][:, kc, :],
                                                               start=(kc == 0), stop=(kc == KC - 1)),
                         r=[bcs, baw2[sl]], w=[bPS[7]] if kc == 0 else (), pw=[bPS[7]] if kc else (), sig=(kc == KC - 1))
                P.op("dve", lambda h, sl=sl: h.tensor_tensor(out=rowsb2[sl], in0=bank(7)[0:1, 0:256], in1=abrow2[sl], op=ALU.add),
                     r=[bPS[7], bab2[sl]], w=[brow2[sl]])
                P.dma("sp", modrow[0:1, c0_:c0_ + 256], rowsb2[sl], srow2[sl], r=[brow2[sl]], pw=[buf("modrow")])
                for c in range(2):
                    P.op("pe", lambda h, c=c, sl=sl: h.matmul(bank(7)[:, 256 + c:257 + c], lhsT=rowsb2[sl][0:1, c * 128:(c + 1) * 128],
                                                             rhs=ones_f[0:1, 0:1], start=True, stop=True),
                         r=[brow2[sl]], w=[bPS[7]] if c == 0 else (), pw=[bPS[7]] if c else (), sig=(c == 1))
                P.op("act", lambda h, g=g: h.activation(out=modT[:, g * 2:(g + 1) * 2], in_=bank(7)[:, 256:258], func=AF.Copy),
                     r=[bPS[7]], pw=[bmodT])

            def spill_segment(c0, nchunks, dst_d, kind):
                ngr = (nchunks + 1) // 2
                for gi in range(ngr):
                    ncols = min(256, (nchunks - gi * 2) * 128)
                    sl = load_w(w_in_v, c0 + gi * 256, ncols)
                    for ci in range(ncols // 128):
                        ch = gi * 2 + ci
                        st = stctr[0] % 2
                        stctr[0] += 1
                        if kind == "gu":
                            fi = ch % 2
                            P.dma("sp", ftc[fi], FT_d[:, :, ch, :].rearrange("tb p t -> p tb t"), sftc[fi], r=[bFT], w=[bftc[fi]])

                        def ev(tci, t0, tl, pb, st=st, ch=ch, first=[True]):
                            dst = stage[st][:, t0:t0 + tl]
                            wl = [bstage[st]] if tci == 0 else []
                            pl = [bstage[st]] if tci else []
                            if kind == "q":
                                qk_evac(pb, tl, gq, dst, pl, src_lo=0, wl=wl)
                            elif kind == "qi":
                                if tci % 2 == 0:
                                    P.op("act", lambda h: h.activation(out=dst, in_=bank(pb)[:, 0:tl], func=AF.Copy), r=[bPS[pb]], w=wl, pw=pl)
                                else:
                                    P.op("dve", lambda h: h.tensor_copy(out=dst, in_=bank(pb)[:, 0:tl]), r=[bPS[pb]], w=wl, pw=pl)
                            elif kind == "sig":
                                P.op("act", lambda h: h.activation(out=dst, in_=bank(pb)[:, 0:tl], func=AF.Sigmoid), r=[bPS[pb]], w=wl, pw=pl)
                            elif kind == "gu":
                                gi_ = gctr[0] % 2
                                gctr[0] += 1
                                fi = ch % 2
                                P.op("act", lambda h: h.activation(out=gtmp[gi_][:, 0:tl], in_=bank(pb)[:, 0:tl], func=AF.Gelu),
                                     r=[bPS[pb]], w=[bgt[gi_]])
                                fsrc = ftc[fi].rearrange("p a b -> p (a b)")[:, t0:t0 + tl]
                                P.op("dve", lambda h: h.tensor_tensor(out=dst, in0=gtmp[gi_][:, 0:tl], in1=fsrc, op=ALU.mult),
                                     r=[bgt[gi_], bftc[fi]], w=wl, pw=pl)
                        fm_chunk(sl, ci * 128, hT, FMC, ev, [bhT])
                        P.dma("sp", dst_d[ch], stage[st], sstage[st], r=[bstage[st]], pw=[buf(kind + "_d")])

            spill_segment(C_GU, 16, ybT_d, "gu")
            byb = buf("gu_d")
            spill_segment(C_Q, 16, qT_d, "q")
            spill_segment(C_QI, 16, qiT_d, "qi")
            sig_d = buf("sig_d")
            spill_segment(C_GA, 32, sa_d, "sig")
            spill_segment(C_GB, 32, sb_d, "sig")
            P.barrier()

            DUMP["wabs"] = wabs
            DUMP["wsgn"] = wsgn
            DUMP["qT_d"] = qT_d
            DUMP["qiT_d"] = qiT_d
            DUMP["ybT_d"] = ybT_d
            DUMP["sa_d"] = sa_d
            DUMP["sb_d"] = sb_d
            DUMP["FT_d"] = FT_d
            DUMP["kT"] = kT
            DUMP["V"] = Vt
            DUMP["kiT"] = kiT
            ckpt(2)
            AA = Alloc(PH0)
            qTs = [AA.get([128, 16, 128], BF16) for _ in range(2)]
            aw3 = [AA.get([128, KC, 128], BF16) for _ in range(2)]
            abrow3 = [AA.get([1, 128], F32) for _ in range(2)]
            rowsb3 = [AA.get([1, 128], F32) for _ in range(2)]
            assert AA.off <= PH0 + 16 * TE * 2
            AA.off = PH0 + 16 * TE * 2
            yaT = AA.get([128, 16, TE], BF16)
            YAT_OFF = PH0 + 16 * TE * 2
            tmd = AA.get([128, 2048], F32)
            acc = AA.get([128, 2048], F32)
            work = AA.get([128, 2048], F32)
            distm = AA.get([128, 2048], F32)
            rbuf = [AA.get([128, 1024], BF16) for _ in range(2)]
            dg = AA.get([128, 32, 128], BF16)
            bdg = buf("dg")
            scb = [AA.get([128, 2048], F32) for _ in range(2)]
            Pb = [AA.get([128, 2048], BF16) for _ in range(2)]
            PT = [AA.get([128, 16, 128], BF16) for _ in range(2)]
            ya = AA.get([128, 2048], BF16)
            m8 = AA.get([128, 8], F32)
            thr = AA.get([128, 8], F32)
            stat = AA.get([128, 64], F32)
            assert AA.off <= ARENA, AA.off
            sq_ = P.sem()
            bq = buf("qT")
            bqi = [buf("qi%d" % i) for i in range(9)]
            for c in range(16):
                P.dma("sp", yaT[:, c, :], qiT_d[c], sq_, r=[buf("qi_d")], pw=bqi)
            bqs = [buf("qTs0"), buf("qTs1")]
            sqs = [P.sem(), P.sem()]

            def load_q(qb):
                tok0_ = QB[qb][0]
                P.dma("sp", qTs[qb % 2], qT_d[:, :, tok0_:tok0_ + 128].rearrange("c p t -> p c t"), sqs[qb % 2], r=[buf("q_d")], w=[bqs[qb % 2]])

            baw3 = [buf("aw3_0"), buf("aw3_1")]
            bab3 = [buf("ab3_0"), buf("ab3_1")]
            brow3 = [buf("row3_0"), buf("row3_1")]
            saw3 = [P.sem(), P.sem()]
            sab3 = [P.sem(), P.sem()]
            srow3 = [P.sem(), P.sem()]
            modq3 = list(range(64, 192))

            def mod_step3():
                if not modq3:
                    return
                g = modq3.pop(0)
                sl = g % 2
                c0_ = g * 128
                P.dma("pool", aw3[sl], ada_w_v[:, :, c0_:c0_ + 128], saw3[sl], w=[baw3[sl]])
                P.dma("sp", abrow3[sl], ada_b[0:1, c0_:c0_ + 128], sab3[sl], w=[bab3[sl]])
                for kc in range(KC):
                    P.op("pe", lambda h, kc=kc, sl=sl: h.matmul(bank(7)[0:1, 0:128], lhsT=cs_t[:, kc:kc + 1], rhs=aw3[sl][:, kc, :],
                                                               start=(kc == 0), stop=(kc == KC - 1)),
                         r=[bcs, baw3[sl]], w=[bPS[7]] if kc == 0 else (), pw=[bPS[7]] if kc else (), sig=(kc == KC - 1))
                P.op("dve", lambda h, sl=sl: h.tensor_tensor(out=rowsb3[sl], in0=bank(7)[0:1, 0:128], in1=abrow3[sl], op=ALU.add),
                     r=[bPS[7], bab3[sl]], w=[brow3[sl]])
                P.dma("sp", modrow[0:1, c0_:c0_ + 128], rowsb3[sl], srow3[sl], r=[brow3[sl]], pw=[buf("modrow")])
                P.op("pe", lambda h, sl=sl: h.matmul(bank(7)[:, 256:257], lhsT=rowsb3[sl][0:1, 0:128], rhs=ones_f[0:1, 0:1], start=True, stop=True),
                     r=[brow3[sl]], w=[bPS[7]])
                P.op("act", lambda h, g=g: h.activation(out=modT[:, g:g + 1], in_=bank(7)[:, 256:257], func=AF.Copy),
                     r=[bPS[7]], pw=[bmodT])
            P.dma("sp", tmd, c_tmd, sq_, w=[buf("tmd")])
            P.barrier()
            bacc, bwork, bdist, bthr = buf("acc"), buf("work"), buf("distm"), buf("thr")
            brb = [buf("rb0"), buf("rb1")]
            bsc = [buf("sc0"), buf("sc1")]
            bP = [buf("P0"), buf("P1")]
            bPT = [buf("PT0"), buf("PT1")]
            bya = buf("ya")
            bst = [buf("st0"), buf("st1")]
            rctr = [0]
            QB = [(0, 896.0, 1024, 7)] + [(128 + 128 * j, 1024.0 + 128 * j, 1024 + 128 * (j + 1), 8 + j) for j in range(8)]
            AB = [acc, distm]
            bAB = [bacc, bdist]

            def stage_A(qb):
                tok0, cst, Sk, dkb = QB[qb]
                accq, baccq = AB[qb % 2], bAB[qb % 2]
                for hh in range(32):
                    P.op("dve", lambda h, hh=hh, qb=qb: h.tensor_scalar(out=dg[:, hh, :], in0=ident, scalar1=wsgn[:, qb, hh:hh + 1], scalar2=None, op0=ALU.mult),
                         r=[bwi, bconst], w=[bdg] if hh == 0 else (), pw=[bdg] if hh else ())
                halves = [(0, 1024)] + ([(1024, Sk - 1024)] if Sk > 1024 else [])
                for (k0, kn) in halves:
                    nch = (kn + 511) // 512

                    def acc_mm(hh, ri, k0=k0, kn=kn):
                        for s0_ in range(0, kn, 512):
                            sn = min(512, kn - s0_)
                            ab = 4 + s0_ // 512
                            P.op("pe", lambda h, hh=hh, ri=ri, s0_=s0_, sn=sn, ab=ab: h.matmul(bank(ab)[:, 0:sn], lhsT=dg[:, hh, :], rhs=rbuf[ri][:, s0_:s0_ + sn],
                                                                                        start=(hh == 0), stop=(hh == 31)),
                                 r=[brb[ri], bdg], w=[bPS[ab]] if hh == 0 else (), pw=[bPS[ab]] if hh else (), sig=True)
                    prev = None
                    for hh in range(32):
                        c, hf = hh // 2, hh % 2
                        pbase = 2 * (rctr[0] % 2)
                        ri = rctr[0] % 2
                        rctr[0] += 1
                        lps = psum[:, pbase * 512:pbase * 512 + kn]
                        for s0_ in range(0, kn, 512):
                            sn = min(512, kn - s0_)
                            bi = pbase + s0_ // 512
                            P.op("pe", lambda h, c=c, hf=hf, s0_=s0_, sn=sn, bi=bi, k0=k0, tok0=tok0: h.matmul(
                                bank(bi)[:, 0:sn], lhsT=yaT[hf * 64:(hf + 1) * 64, c, tok0:tok0 + 128],
                                rhs=kiT[hf * 64:(hf + 1) * 64, k0 + s0_:k0 + s0_ + sn], start=True, stop=True),
                                r=[bqi[qb], bkiT], w=[bPS[bi]])
                        rbs = [bPS[pbase + i] for i in range(nch)]
                        P.op("act", lambda h, lps=lps, ri=ri, kn=kn, hh=hh, qb=qb: h.activation(out=rbuf[ri][:, 0:kn], in_=lps, func=AF.Relu,
                                                                                           scale=wabs[:, qb, hh:hh + 1]),
                             r=rbs + [bwi], w=[brb[ri]])
                        if prev is not None:
                            acc_mm(*prev)
                        prev = (hh, ri)
                    acc_mm(*prev)
                    aps = psum[:, 4 * 512:4 * 512 + kn]
                    abufs = [bPS[4 + i] for i in range(nch)]
                    if k0 == 0:
                        P.op("dve", lambda h, aps=aps, kn=kn: h.tensor_scalar(out=accq[:, 0:kn], in0=aps, scalar1=ctxm, scalar2=None, op0=ALU.add),
                             r=abufs, pw=[baccq])
                    else:
                        P.op("dve", lambda h, aps=aps, kn=kn, k0=k0: h.tensor_copy(out=accq[:, k0:k0 + kn], in_=aps), r=abufs, pw=[baccq])

            def stage_B(qb):
                tok0, cst, Sk, dkb = QB[qb]
                accq, baccq = AB[qb % 2], bAB[qb % 2]
                ops = []
                d0 = dkb * 128
                ops.append(lambda: P.op("dve", lambda h: h.tensor_tensor(out=accq[:, d0:d0 + 128], in0=accq[:, d0:d0 + 128], in1=cmask, op=ALU.add),
                                        r=[baccq], w=[baccq]))
                for it in range(32):
                    src = accq if it == 0 else work
                    ops.append(lambda src=src: P.op("dve", lambda h: h.max(out=m8, in_=src[:, 0:Sk]), r=[baccq, bwork], w=[bthr]))
                    if it < 31:
                        ops.append(lambda src=src: P.op("dve", lambda h: h.match_replace(out=work[:, 0:Sk], in_to_replace=m8, in_values=src[:, 0:Sk],
                                                                                      imm_value=-3.0e38), r=[bthr, baccq, bwork], w=[bwork]))
                ops.append(lambda: P.op("dve", lambda h: h.tensor_scalar(out=thr[:, 0:1], in0=m8[:, 7:8], scalar1=-1.0e29, scalar2=None, op0=ALU.max),
                                        r=[bthr], w=[bthr]))
                ops.append(lambda: P.op("dve", lambda h: h.tensor_scalar(out=work[:, 0:Sk], in0=accq[:, 0:Sk], scalar1=thr[:, 0:1], scalar2=-BIGD,
                                                                       op0=ALU.is_ge, op1=ALU.mult), r=[bthr, baccq], w=[bwork]))
                ops.append(lambda: P.op("dve", lambda h: h.scalar_tensor_tensor(out=accq[:, 0:Sk], in0=tmd[:, 0:Sk], scalar=cst + BIGD,
                                                                              in1=work[:, 0:Sk], op0=ALU.add, op1=ALU.add),
                                        r=[bwork, buf("tmd")], w=[baccq]))
                return ops

            def stage_C(qb, filler):
                tok0, cst, Sk, dkb = QB[qb]
                accq, baccq = AB[qb % 2], bAB[qb % 2]
                halves = [(0, 1024)] + ([(1024, Sk - 1024)] if Sk > 1024 else [])
                nkb = Sk // 128
                for hd in range(16):
                    g = hd // 4
                    si = hd % 2
                    for hi_, (k0, kn) in enumerate(halves):
                        pbase = 0 if hi_ == 0 else 2
                        for s0_ in range(0, kn, 512):
                            sn = min(512, kn - s0_)
                            bi = pbase + s0_ // 512
                            P.op("pe", lambda h, hd=hd, g=g, s0_=s0_, sn=sn, bi=bi, k0=k0, qb=qb: h.matmul(
                                bank(bi)[:, 0:sn], lhsT=qTs[qb % 2][:, hd, :], rhs=kT[:, g, k0 + s0_:k0 + s0_ + sn],
                                start=True, stop=True), r=[bqs[qb % 2], bkT], w=[bPS[bi]])
                        lps = psum[:, pbase * 512:pbase * 512 + kn]
                        rbs = [bPS[pbase + i] for i in range((kn + 511) // 512)]
                        P.op("dve", lambda h, si=si, k0=k0, kn=kn, lps=lps, hd=hd: h.scalar_tensor_tensor(
                            out=scb[si][:, k0:k0 + kn], in0=accq[:, k0:k0 + kn], scalar=-SLOPES[hd] / SCALE, in1=lps,
                            op0=ALU.mult, op1=ALU.add), r=rbs + [baccq], w=[bsc[si]] if hi_ == 0 else (), pw=[bsc[si]] if hi_ else ())
                    mx = stat[:, si * 4:si * 4 + 1]
                    nmx = stat[:, si * 4 + 1:si * 4 + 2]
                    rsum = stat[:, si * 4 + 2:si * 4 + 3]
                    P.op("dve", lambda h, si=si, Sk=Sk, mx=mx: h.reduce_max(out=mx, in_=scb[si][:, 0:Sk], axis=AX.X), r=[bsc[si]], w=[bst[si]])
                    P.op("dve", lambda h, mx=mx, nmx=nmx: h.tensor_scalar(out=nmx, in0=mx, scalar1=-SCALE, scalar2=None, op0=ALU.mult),
                         r=[bst[si]], w=[bst[si]])
                    P.op("act", lambda h, si=si, Sk=Sk, nmx=nmx, rsum=rsum: h.activation(out=Pb[si][:, 0:Sk], in_=scb[si][:, 0:Sk], func=AF.Exp,
                                                                                      bias=nmx, scale=SCALE, accum_out=rsum),
                         r=[bsc[si], bst[si]], w=[bP[si], buf("rsum%d" % si)])
                    for kb in range(nkb):
                        pb = 4 + kb // 8
                        P.op("pe", lambda h, si=si, kb=kb, pb=pb: h.transpose(bank_bf(pb)[:, (kb % 8) * 128:(kb % 8 + 1) * 128],
                                                                            Pb[si][:, kb * 128:(kb + 1) * 128], ident),
                             r=[bP[si]], w=[bPS[pb]] if kb % 8 == 0 else (), pw=[bPS[pb]] if kb % 8 else (),
                             sig=(kb % 8 == 7 or kb == nkb - 1))
                    for b2 in range((nkb + 7) // 8):
                        n8 = min(8, nkb - b2 * 8)
                        srcp = bank_bf(4 + b2)[:, 0:n8 * 128].rearrange("p (a b) -> p a b", a=n8)
                        if b2 == 0:
                            P.op("act", lambda h, si=si, b2=b2, n8=n8, srcp=srcp: h.activation(out=PT[si][:, b2 * 8:b2 * 8 + n8, :], in_=srcp, func=AF.Copy),
                                 r=[bPS[4 + b2]], w=[bPT[si]])
                        else:
                            P.op("dve", lambda h, si=si, b2=b2, n8=n8, srcp=srcp: h.tensor_copy(out=PT[si][:, b2 * 8:b2 * 8 + n8, :], in_=srcp),
                                 r=[bPS[4 + b2]], pw=[bPT[si]])
                    ob = 6
                    for kb in range(nkb):
                        P.op("pe", lambda h, si=si, kb=kb, g=g, ob=ob, nkb=nkb: h.matmul(bank(ob)[:, 0:128], lhsT=PT[si][:, kb, :],
                                                                              rhs=Vt[:, kb, g * 128:(g + 1) * 128], start=(kb == 0), stop=(kb == nkb - 1)),
                             r=[bPT[si], bV], w=[bPS[ob]] if kb == 0 else (), pw=[bPS[ob]] if kb else (), sig=(kb == nkb - 1))
                    rinv = stat[:, si * 4 + 3:si * 4 + 4]
                    P.op("dve", lambda h, rsum=rsum, rinv=rinv: h.reciprocal(out=rinv, in_=rsum), r=[buf("rsum%d" % si)], w=[buf("rinv%d" % si)])
                    P.op("act", lambda h, hd=hd, ob=ob, rinv=rinv: h.activation(out=ya[:, hd * 128:(hd + 1) * 128], in_=bank(ob)[:, 0:128],
                                                                               func=AF.Copy, scale=rinv),
                         r=[bPS[ob], buf("rinv%d" % si)], pw=[bya])
                    filler(5)
                    mod_step3()
                for b2 in range(2):
                    for j in range(8):
                        hd = b2 * 8 + j
                        P.op("pe", lambda h, hd=hd, j=j, b2=b2: h.transpose(bank_bf(4 + b2)[:, j * 128:(j + 1) * 128], ya[:, hd * 128:(hd + 1) * 128], ident),
                             r=[bya], w=[bPS[4 + b2]] if j == 0 else (), pw=[bPS[4 + b2]] if j else (), sig=(j == 7))
                    srcp = bank_bf(4 + b2).rearrange("p (a b) -> p a b", a=8)
                    P.op("dve", lambda h, b2=b2, tok0=tok0, srcp=srcp: h.tensor_copy(out=yaT[:, b2 * 8:(b2 + 1) * 8, tok0:tok0 + 128], in_=srcp),
                         r=[bPS[4 + b2]], w=[bqi[qb]] if b2 == 0 else (), pw=[bqi[qb]] if b2 else ())

            NQ = len(QB)
            load_q(0)
            stage_A(0)
            for t_ in stage_B(0):
                t_()
            if NQ > 1:
                stage_A(1)
            for qb in range(NQ):
                pend = stage_B(qb + 1) if qb + 1 < NQ else []

                def filler(n, pend=pend):
                    for _ in range(n):
                        if pend:
                            pend.pop(0)()
                if qb + 1 < NQ:
                    load_q(qb + 1)
                stage_C(qb, filler)
                while pend:
                    pend.pop(0)()
                if qb + 2 < NQ:
                    stage_A(qb + 2)
            while modq3:
                mod_step3()
            P.op("dve", lambda h: h.scalar_tensor_tensor(out=a2, in0=sc2, scalar=1.0, in1=n2g_t, op0=ALU.add, op1=ALU.mult),
                 r=[bmodT], pw=[bmodT])
            P.barrier()

            DUMP["yaT"] = yaT
            ckpt(3)
            AM = Alloc(KV0)
            mT = AM.get([128, KC, TE], BF16)
            assert AM.off <= YAT_OFF, (AM.off, YAT_OFF)
            AM2 = Alloc(YAT_OFF + 16 * TE * 2)
            ybT = AM2.get([128, 16, TE], BF16)
            wab = [[AM2.get([128, 16, 128], BF16) for _ in range(2)] for _ in range(2)]
            gst = [[AM2.get([128, TE], BF16) for _ in range(2)] for _ in range(2)]
            mtmp = [AM2.get([128, 344], F32) for _ in range(2)]
            assert AM2.off <= ARENA, AM2.off
            byb2 = buf("ybT")
            syb = P.sem()
            for c in range(16):
                P.dma("sp", ybT[:, c, :], ybT_d[c], syb, r=[buf("gu_d")], pw=[byb2])
            P.barrier()
            bwab = [[buf("wab%d%d" % (a, i)) for i in range(2)] for a in range(2)]
            swab = [[P.sem() for i in range(2)] for a in range(2)]
            bgst = [[buf("gst%d%d" % (a, i)) for i in range(2)] for a in range(2)]
            sgst = [[P.sem() for i in range(2)] for a in range(2)]
            bmt = [buf("mt0"), buf("mt1")]
            bmT = buf("mT")
            P.op("dve", lambda h: h.memset(mT[:, :, 0:128], 0.0), w=[bmT])
            mctr = [0]
            for j in range(32):
                sl = j % 2
                P.dma("pool", wab[0][sl], w_a_v[:, :, j * 128:(j + 1) * 128], swab[0][sl], w=[bwab[0][sl]])
                P.dma("pool", wab[1][sl], w_b_v[:, :, j * 128:(j + 1) * 128], swab[1][sl], w=[bwab[1][sl]])
                P.dma("sp", gst[0][sl], sa_d[j], sgst[0][sl], r=[buf("sig_d")], w=[bgst[0][sl]])
                P.dma("sp", gst[1][sl], sb_d[j], sgst[1][sl], r=[buf("sig_d")], w=[bgst[1][sl]])
                for tci, (t0, tl) in enumerate(FMC):
                    pa = next_ps()
                    pbb = next_ps()
                    for kc in range(16):
                        P.op("pe", lambda h, kc=kc, pa=pa, t0=t0, tl=tl, sl=sl: h.matmul(bank(pa)[:, 0:tl], lhsT=wab[0][sl][:, kc, :], rhs=yaT[:, kc, t0:t0 + tl],
                                                                                     start=(kc == 0), stop=(kc == 15)),
                             r=[bwab[0][sl]] + bqi, w=[bPS[pa]] if kc == 0 else (), pw=[bPS[pa]] if kc else (), sig=(kc == 15))
                    for kc in range(16):
                        P.op("pe", lambda h, kc=kc, pbb=pbb, t0=t0, tl=tl, sl=sl: h.matmul(bank(pbb)[:, 0:tl], lhsT=wab[1][sl][:, kc, :], rhs=ybT[:, kc, t0:t0 + tl],
                                                                                       start=(kc == 0), stop=(kc == 15)),
                             r=[bwab[1][sl], byb2], w=[bPS[pbb]] if kc == 0 else (), pw=[bPS[pbb]] if kc else (), sig=(kc == 15))
                    mi = mctr[0] % 2
                    mctr[0] += 1
                    P.op("dve", lambda h, mi=mi, pa=pa, t0=t0, tl=tl, sl=sl: h.tensor_tensor(out=mtmp[mi][:, 0:tl], in0=bank(pa)[:, 0:tl], in1=gst[0][sl][:, t0:t0 + tl], op=ALU.mult),
                         r=[bPS[pa], bgst[0][sl]], w=[bmt[mi]])
                    P.op("dve", lambda h, mi=mi, pbb=pbb, t0=t0, tl=tl, sl=sl: h.tensor_tensor(out=bank(pbb)[:, 0:tl], in0=bank(pbb)[:, 0:tl], in1=gst[1][sl][:, t0:t0 + tl], op=ALU.mult),
                         r=[bPS[pbb], bgst[1][sl]], w=[bPS[pbb]])
                    P.op("dve", lambda h, mi=mi, pbb=pbb, t0=t0, tl=tl, j=j: h.tensor_tensor(out=mT[:, j, t0:t0 + tl], in0=bank(pbb)[:, 0:tl], in1=mtmp[mi][:, 0:tl], op=ALU.add),
                         r=[bPS[pbb], bmt[mi]], pw=[bmT])
            P.barrier()

            DUMP["mT"] = mT
            ckpt(4)
            AO = Alloc(KV0 + KC * TE * 2)
            wo = [AO.get([128, KC, 256], BF16) for _ in range(2)]
            g1b = AO.get([128, D], F32)
            xp = [AO.get([128, 256], F32) for _ in range(3)]
            assert AO.off <= ARENA
            bwo = [buf("wo0"), buf("wo1")]
            swo = [P.sem(), P.sem()]
            bxp = [buf("xp0"), buf("xp1"), buf("xp2")]
            sxp = [P.sem(), P.sem(), P.sem()]
            sxo = [P.sem(), P.sem(), P.sem()]
            bg1b = buf("g1b")
            P.dma("sp", g1b, modrow[0:1, 2 * D:3 * D].partition_broadcast(128), P.sem(), r=[buf("modrow")], w=[bg1b])
            bxm = buf("xmid_d")
            xctr = [0]
            for cg in range(16):
                sl = cg % 2
                P.dma("pool", wo[sl], w_out_v[:, :, cg * 256:(cg + 1) * 256], swo[sl], w=[bwo[sl]])
                for tb in range(9):
                    xi = xctr[0] % 3
                    xctr[0] += 1
                    P.dma("sp", xp[xi], x_ext[tb * 128:(tb + 1) * 128, cg * 256:(cg + 1) * 256], sxp[xi], w=[bxp[xi]])
                    pb = next_ps()
                    for kc in range(KC):
                        P.op("pe", lambda h, kc=kc, pb=pb, tb=tb, sl=sl: h.matmul(bank(pb)[:, 0:256], lhsT=mT[:, kc, tb * 128:(tb + 1) * 128], rhs=wo[sl][:, kc, :],
                                                                                start=(kc == 0), stop=(kc == KC - 1)),
                             r=[bmT, bwo[sl]], w=[bPS[pb]] if kc == 0 else (), pw=[bPS[pb]] if kc else (), sig=(kc == KC - 1))
                    P.op("dve", lambda h, pb=pb, cg=cg: h.tensor_tensor(out=bank(pb)[:, 0:256], in0=bank(pb)[:, 0:256], in1=g1b[:, cg * 256:(cg + 1) * 256], op=ALU.mult),
                         r=[bPS[pb], bg1b], w=[bPS[pb]])
                    P.op("dve", lambda h, pb=pb, xi=xi: h.tensor_tensor(out=xp[xi], in0=bank(pb)[:, 0:256], in1=xp[xi], op=ALU.add),
                         r=[bPS[pb], bxp[xi]], w=[bxp[xi]])
                    P.dma("act", xmid_d[tb * 128:(tb + 1) * 128, cg * 256:(cg + 1) * 256], xp[xi], sxo[xi], r=[bxp[xi]], pw=[bxm])
            P.barrier()

            DUMP["xmid_d"] = xmid_d
            ckpt(5)
            h2T = sb(HT0, [128, KC, 1026], BF16)
            xm_blocks = [(0, 126, 128, 0)] + [(128 + b * 128, 0, 128, 2 + b * 128) for b in range(8)]
            norm_phase(xmid_d, xm_blocks, a2, sh2, h2T)
            P.barrier()

            DUMP["h2T"] = h2T
            ckpt(6)
            H2END = HT0 + KC * 1026 * 2
            AU = Alloc((H2END + 31) // 32 * 32)
            wu = [AU.get([128, KC, 2, 128], BF16) for _ in range(2)]
            araw = [[AU.get([128, 1026], F32) for _ in range(2)] for _ in range(2)]
            cacc = [AU.get([128, 1024], F32) for _ in range(2)]
            sgl = AU.get([128, 1024], F32)
            gout = [AU.get([128, 1024], BF16) for _ in range(2)]
            assert AU.off <= ARENA, AU.off
            bwu = [buf("wu0"), buf("wu1")]
            swu = [P.sem(), P.sem()]
            bar = [[buf("ar%d%d" % (a, i)) for i in range(2)] for a in range(2)]
            bca = [buf("ca0"), buf("ca1")]
            bsgl = buf("sgl")
            bgo = [buf("go0"), buf("go1")]
            sgo = [P.sem(), P.sem()]
            bh2 = bhT
            UPC = [(0, 342), (342, 342), (684, 342)]
            bgat = buf("gat_d")
            for j in range(NFF):
                sl = j % 2
                P.dma("pool", wu[sl][:, :, 0, :], w_up_v[:, :, j * 128:(j + 1) * 128], swu[sl], w=[bwu[sl]])
                P.dma("pool", wu[sl][:, :, 1, :], w_up_v[:, :, DFF + j * 128:DFF + (j + 1) * 128], swu[sl], pw=[bwu[sl]])
                for gv_ in range(2):
                    ai = j % 2
                    for tci, (t0, tl) in enumerate(UPC):
                        pb = (gv_ * 3 + tci) if True else 0
                        for kc in range(KC):
                            P.op("pe", lambda h, kc=kc, pb=pb, t0=t0, tl=tl, sl=sl, gv_=gv_: h.matmul(bank(pb)[:, 0:tl], lhsT=wu[sl][:, kc, gv_, :], rhs=h2T[:, kc, t0:t0 + tl],
                                                                                              start=(kc == 0), stop=(kc == KC - 1)),
                                 r=[bwu[sl], bh2], w=[bPS[pb]] if kc == 0 else (), pw=[bPS[pb]] if kc else (), sig=(kc == KC - 1))
                        if tci % 2 == 0:
                            P.op("act", lambda h, pb=pb, t0=t0, tl=tl, gv_=gv_, ai=ai: h.activation(out=araw[gv_][ai][:, t0:t0 + tl], in_=bank(pb)[:, 0:tl], func=AF.Copy),
                                 r=[bPS[pb]], w=[bar[gv_][ai]] if tci == 0 else (), pw=[bar[gv_][ai]] if tci else ())
                        else:
                            P.op("dve", lambda h, pb=pb, t0=t0, tl=tl, gv_=gv_, ai=ai: h.tensor_copy(out=araw[gv_][ai][:, t0:t0 + tl], in_=bank(pb)[:, 0:tl]),
                                 r=[bPS[pb]], pw=[bar[gv_][ai]])
                    col = gv_ * NFF + j
                    ar = araw[gv_][ai]
                    P.op("dve", lambda h, ar=ar: h.tensor_scalar(out=ar[:, 0:2], in0=ar[:, 0:2], scalar1=hflag, scalar2=None, op0=ALU.mult),
                         r=[bar[gv_][ai]], w=[bar[gv_][ai]])
                    w0, w1, w2 = cw_t[:, col:col + 1], cw_t[:, 172 + col:172 + col + 1], cw_t[:, 344 + col:344 + col + 1]
                    cbv = cb_t[:, col:col + 1]
                    if gv_ == 0:
                        P.op("dve", lambda h, ar=ar, w2=w2, cbv=cbv, gv_=gv_: h.tensor_scalar(out=cacc[gv_], in0=ar[:, 2:1026], scalar1=w2, scalar2=cbv, op0=ALU.mult, op1=ALU.add),
                             r=[bar[gv_][ai]], w=[bca[gv_]])
                    else:
                        P.op("act", lambda h, ar=ar, w2=w2, cbv=cbv, gv_=gv_: h.activation(out=cacc[gv_], in_=ar[:, 2:1026], func=AF.Identity, scale=w2, bias=cbv),
                             r=[bar[gv_][ai]], w=[bca[gv_]])
                    P.op("dve", lambda h, ar=ar, w1=w1, gv_=gv_: h.scalar_tensor_tensor(out=cacc[gv_], in0=ar[:, 1:1025], scalar=w1, in1=cacc[gv_], op0=ALU.mult, op1=ALU.add),
                         r=[bar[gv_][ai], bca[gv_]], w=[bca[gv_]])
                    P.op("dve", lambda h, ar=ar, w0=w0, gv_=gv_: h.scalar_tensor_tensor(out=cacc[gv_], in0=ar[:, 0:1024], scalar=w0, in1=cacc[gv_], op0=ALU.mult, op1=ALU.add),
                         r=[bar[gv_][ai], bca[gv_]], w=[bca[gv_]])
                P.op("act", lambda h: h.activation(out=sgl, in_=cacc[0], func=AF.Silu), r=[bca[0]], w=[bsgl])
                gi = j % 2
                P.op("dve", lambda h, gi=gi: h.tensor_tensor(out=gout[gi], in0=sgl, in1=cacc[1], op=ALU.mult), r=[bsgl, bca[1]], w=[bgo[gi]])
                P.dma("sp", gat_d[:, :, j, :].rearrange("tb p t -> p tb t"), gout[gi].rearrange("p (tb t) -> p tb t", tb=8), sgo[gi], r=[bgo[gi]], pw=[bgat])
            P.barrier()

            DUMP["gat_d"] = gat_d
            ckpt(7)
            AD = Alloc(KV0)
            wd = [AD.get([128, 43, 512], BF16) for _ in range(2)]
            gp = [AD.get([128, 43, 128], BF16) for _ in range(3)]
            g2b = AD.get([128, D], F32)
            xo = [AD.get([128, 512], F32) for _ in range(3)]
            assert AD.off <= ARENA, AD.off
            bwd = [buf("wd0"), buf("wd1")]
            swd = [P.sem(), P.sem()]
            bgp = [buf("gp0"), buf("gp1"), buf("gp2")]
            sgp = [P.sem(), P.sem(), P.sem()]
            bxo = [buf("xo0"), buf("xo1"), buf("xo2")]
            sxi = [P.sem(), P.sem(), P.sem()]
            sxw = [P.sem(), P.sem(), P.sem()]
            bg2b = buf("g2b")
            P.dma("sp", g2b, modrow[0:1, 5 * D:6 * D].partition_broadcast(128), P.sem(), r=[buf("modrow")], w=[bg2b])
            gctr2 = [0]
            octr = [0]
            bout = buf("out")
            for cg in range(8):
                for hf in range(2):
                    P.dma("pool", wd[hf], w_down_v[:, hf * 43:(hf + 1) * 43, cg * 512:(cg + 1) * 512], swd[hf], w=[bwd[hf]])
                    for tb in range(8):
                        gi = gctr2[0] % 3
                        gctr2[0] += 1
                        P.dma("sp", gp[gi], gat_d[tb, :, hf * 43:(hf + 1) * 43, :], sgp[gi], r=[bgat], w=[bgp[gi]])
                        for kc in range(43):
                            P.op("pe", lambda h, kc=kc, tb=tb, hf=hf, gi=gi: h.matmul(bank(tb), lhsT=gp[gi][:, kc, :], rhs=wd[hf][:, kc, :],
                                                                                    start=(hf == 0 and kc == 0), stop=(hf == 1 and kc == 42)),
                                 r=[bgp[gi], bwd[hf]], w=[bPS[tb]] if (hf == 0 and kc == 0) else (), pw=() if (hf == 0 and kc == 0) else [bPS[tb]],
                                 sig=(kc == 42))
                        if hf == 1:
                            oi = octr[0] % 3
                            octr[0] += 1
                            P.dma("sp", xo[oi], xmid_d[128 + tb * 128:128 + (tb + 1) * 128, cg * 512:(cg + 1) * 512], sxi[oi], r=[bxm], w=[bxo[oi]])
                            P.op("dve", lambda h, tb=tb, cg=cg: h.tensor_tensor(out=bank(tb), in0=bank(tb), in1=g2b[:, cg * 512:(cg + 1) * 512], op=ALU.mult),
                                 r=[bPS[tb], bg2b], w=[bPS[tb]])
                            P.op("dve", lambda h, tb=tb, oi=oi: h.tensor_tensor(out=xo[oi], in0=bank(tb), in1=xo[oi], op=ALU.add),
                                 r=[bPS[tb], bxo[oi]], w=[bxo[oi]])
                            P.dma("act", out_d[tb * 128:(tb + 1) * 128, cg * 512:(cg + 1) * 512], xo[oi], sxw[oi], r=[bxo[oi]], pw=[bout])
            P.barrier()

        except _Stop:
            pass
        if dbg:
            P.barrier()
            sdb = P.sem()
            for name, shape, dt in dbg:
                src = DUMP[name]
                P.dma("sp", dbg_outs[name], src, sdb)
            P.barrier()
        if stop_after is not None:
            sfin = P.sem()
            P.dma("sp", out_d[0:128, 0:128], ones_f, sfin)
            P.barrier()
        print("ops:", {e: len(P.ops[e]) for e in ENGS}, "dma sems:", P.nsem, flush=True)
        with nc.Block() as block:
            P.emit(block)
    return nc


dbg_qb = [8]
NORM_MODE = 0


def _consts():
    t = np.arange(128)[:, None]
    s = np.arange(128)[None, :]
    ident = np.eye(128, dtype=np.float32)
    tril = (s <= t).astype(np.float32)
    cmask = np.where(s <= t, 0.0, NEG).astype(np.float32)
    tmd = (np.arange(128)[:, None] - np.arange(2048)[None, :]).astype(np.float32)
    return ident, tril, cmask, tmd


_NC_CACHE = {}


def make_in_maps(x, c, ada_w, ada_b, norm1_g, w_in, q_norm_g, k_norm_g, sgu_norm_g, sgu_w, sgu_b,
                 w_branch_a, w_branch_b, w_out, norm2_g, w_up, conv_w, conv_b, w_down):
    f = lambda a: np.ascontiguousarray(np.asarray(a, dtype=np.float32))
    x = f(x); c = f(c)
    ident, tril, cmask, tmd = _consts()
    shared = {
        "ada_w": f(ada_w[0]), "ada_b": f(ada_b[0]).reshape(1, -1),
        "n1g": f(np.asarray(norm1_g[0]).reshape(KC, 128).T), "n2g": f(np.asarray(norm2_g[0]).reshape(KC, 128).T),
        "w_in": f(w_in[0]),
        "gsguT": f(np.asarray(sgu_norm_g[0]).reshape(16, 128).T),
        "sgu_w": f(sgu_w[0]),
        "sgu_bB": f(np.broadcast_to(np.asarray(sgu_b[0]).reshape(1, 1024), (128, 1024))),
        "w_a": f(w_branch_a[0]), "w_b": f(w_branch_b[0]), "w_out": f(w_out[0]), "w_up": f(w_up[0]),
        "convw": f(np.asarray(conv_w[0]).reshape(3, 172, 128).transpose(2, 0, 1).reshape(128, 3 * 172)),
        "convb": f(np.asarray(conv_b[0]).reshape(172, 128).T),
        "w_down": f(w_down[0]),
        "c_ident": ident, "c_tril": tril, "c_cmask": cmask, "c_tmd": tmd,
    }
    gq = np.asarray(q_norm_g[0], dtype=np.float32)
    gk = np.asarray(k_norm_g[0], dtype=np.float32)
    in_maps = []
    for core in range(8):
        b, hf = core // 2, core % 2
        own = x[b, hf * 1024:(hf + 1) * 1024]
        if hf == 1:
            ctx = x[b, 0:1024]
        else:
            ctx = np.zeros((1024, D), np.float32)
        x_ext = np.ascontiguousarray(np.concatenate([ctx[896:1024], own], axis=0))
        sm = np.zeros((128, 8), np.float32)
        sm[:, 0] = gq
        sm[:, 1] = gk
        sm[:, 2] = 0.0 if hf == 1 else NEG
        sm[:, 3] = 1.0 if hf == 1 else 0.0
        sm[:, 4] = EPS
        m = dict(shared)
        m["x_ext"] = x_ext
        m["x_ctx"] = np.ascontiguousarray(ctx)
        m["c_t"] = f(c[b].reshape(KC, 128).T)
        m["smalls"] = sm
        in_maps.append(m)
    return in_maps


PHASE_INPUTS = {"x_ctx": 0.5, "w_in": 0.6, "x_ext": 2, "sgu_bB": 2, "w_a": 4, "w_b": 4, "w_out": 5, "w_up": 7, "w_down": 8}


def filter_inputs(in_maps, stop_after):
    if stop_after is None:
        return in_maps
    return [{k: v for k, v in m.items() if PHASE_INPUTS.get(k, 0) <= stop_after} for m in in_maps]


def kernel(**inputs):
    if "nc" not in _NC_CACHE:
        _NC_CACHE["nc"] = build_nc()
    nc = _NC_CACHE["nc"]
    in_maps = make_in_maps(**inputs)
    res = run_bass_kernel_spmd(nc, in_maps, core_ids=list(range(8)))
    out = np.empty((NB, SEQ, D), np.float32)
    for core in range(8):
        b, hf = core // 2, core % 2
        out[b, hf * 1024:(hf + 1) * 1024] = res.results[core]["out"]
    return out
```
